# Optimizing a Trainium2 kernel written in Bass

```python
import math
import jax, jax.numpy as jnp
from jax import lax
import numpy as np

D_MODEL = 1024
BATCH = 8
SEQ = 4096
DEPTH = 4

N_MIXERS = 2
EPS = 1e-6
NEG_INF = -1e30

GMLP_CHUNK = 128
GMLP_WIDTH = 3 * D_MODEL
GMLP_GROUPS = 8
GMLP_GROUP_WIDTH = GMLP_WIDTH // GMLP_GROUPS

DIL_PATTERNS = ((128, 1), (512, 4), (2048, 16))
N_DIL_GROUPS = len(DIL_PATTERNS)
HEADS_PER_GROUP = 8
HEAD_DIM = 64
ATTN_WIDTH = HEADS_PER_GROUP * HEAD_DIM
QKV_WIDTH = 3 * N_DIL_GROUPS * ATTN_WIDTH
QUERY_BLOCK = 128

N_EXPERTS = 16
CAPACITY_FACTOR = 2
D_EXPERT = 2 * D_MODEL

kernel_name = "hybrid_gmlp_dilated_attn_ec_moe_encoder"


def rmsnorm(x, gain):
    xf = x.astype(jnp.float32)
    y = xf * lax.rsqrt(jnp.mean(xf * xf, axis=-1, keepdims=True) + EPS)
    return (y * gain.astype(jnp.float32)).astype(x.dtype)


def head_rmsnorm(t, gain):
    tf = t.astype(jnp.float32)
    y = tf * lax.rsqrt(jnp.mean(tf * tf, axis=-1, keepdims=True) + EPS)
    return y * gain.astype(jnp.float32)[:, None, :]


def alibi_slopes(n_heads):
    return jnp.exp2(-8.0 * jnp.arange(1, n_heads + 1, dtype=jnp.float32) / n_heads)


def gmlp_mixer(h, w_in, b_in, v_gain, w_s, b_s, w_out, b_out):
    B, S, _ = h.shape
    z = jax.nn.gelu(h @ w_in + b_in)
    u, v = z[..., :GMLP_WIDTH], z[..., GMLP_WIDTH:]
    v = rmsnorm(v, v_gain)
    n_chunks = S // GMLP_CHUNK
    v = v.reshape(B, n_chunks, GMLP_CHUNK, GMLP_GROUPS, GMLP_GROUP_WIDTH)
    v = jnp.einsum('gts,bcsge->bctge', w_s, v) + b_s.T[None, None, :, :, None]
    return (u * v.reshape(B, S, GMLP_WIDTH)) @ w_out + b_out


def dilated_band_attention(q, k, v, dil, half, slopes):
    B, S, H, Dh = q.shape
    L = S // dil
    N = B * dil

    def to_sub(t):
        return t.reshape(B, L, dil, H, Dh).transpose(0, 2, 1, 3, 4).reshape(N, L, H, Dh)

    qs, ks, vs = to_sub(q), to_sub(k), to_sub(v.astype(jnp.float32))
    nb = -(-L // QUERY_BLOCK)
    Lp = nb * QUERY_BLOCK
    KB = QUERY_BLOCK + 2 * half
    qs = jnp.pad(qs, ((0, 0), (0, Lp - L), (0, 0), (0, 0)))
    kpad = ((0, 0), (half, Lp - L + half), (0, 0), (0, 0))
    ks, vs = jnp.pad(ks, kpad), jnp.pad(vs, kpad)
    kidx = jnp.arange(nb)[:, None] * QUERY_BLOCK + jnp.arange(KB)[None, :]
    kb, vb = ks[:, kidx], vs[:, kidx]
    qb = qs.reshape(N, nb, QUERY_BLOCK, H, Dh)
    s = jnp.einsum('nbqhd,nbkhd->nbhqk', qb, kb) / math.sqrt(Dh)
    rel = jnp.arange(KB)[None, :] - half - jnp.arange(QUERY_BLOCK)[:, None]
    kpos = kidx - half
    valid = (jnp.abs(rel)[None] <= half) & ((kpos >= 0) & (kpos < L))[:, None, :]
    bias = -slopes[:, None, None] * (dil * jnp.abs(rel)).astype(jnp.float32)[None]
    s = jnp.where(valid[None, :, None], s + bias[None, None], NEG_INF)
    lse = jax.nn.logsumexp(s, axis=-1)
    p = jnp.exp(s - lse[..., None])
    o = jnp.einsum('nbhqk,nbkhd->nbqhd', p, vb).reshape(N, Lp, H, Dh)[:, :L]
    lse = lse.transpose(0, 1, 3, 2).reshape(N, Lp, H)[:, :L]
    o = o.reshape(B, dil, L, H, Dh).transpose(0, 2, 1, 3, 4).reshape(B, S, H, Dh)
    lse = lse.reshape(B, dil, L, H).transpose(0, 2, 1, 3).reshape(B, S, H)
    return o, lse


def dilated_mixer(h, w_qkv, q_gain, k_gain, w_o):
    B, S, _ = h.shape
    qkv = (h @ w_qkv).reshape(B, S, 3, N_DIL_GROUPS, HEADS_PER_GROUP, HEAD_DIM)
    q = head_rmsnorm(qkv[:, :, 0], q_gain)
    k = head_rmsnorm(qkv[:, :, 1], k_gain)
    v = qkv[:, :, 2]
    slopes = alibi_slopes(HEADS_PER_GROUP)
    outs, lses = [], []
    for g, (window, dil) in enumerate(DIL_PATTERNS):
        o, l = dilated_band_attention(q[:, :, g], k[:, :, g], v[:, :, g], dil, window // (2 * dil), slopes)
        outs.append(o)
        lses.append(l)
    wts = jax.nn.softmax(jnp.stack(lses, 0), axis=0)
    o = jnp.einsum('gbsh,gbshd->bshd', wts, jnp.stack(outs, 0))
    return o.reshape(B, S, ATTN_WIDTH).astype(h.dtype) @ w_o


def ec_moe(h, w_router, b_router, w1, w3, w2):
    B, S, D = h.shape
    cap = CAPACITY_FACTOR * S // N_EXPERTS
    logits = jnp.einsum('bsd,de->bse', h, w_router).astype(jnp.float32) + b_router.astype(jnp.float32)
    aff = jax.nn.softmax(logits, axis=-1)
    gates, idx = lax.top_k(jnp.swapaxes(aff, 1, 2), cap)
    xin = jax.vmap(lambda hb, ib: hb[ib])(h, idx)
    hid = jax.nn.silu(jnp.einsum('becd,edf->becf', xin, w1)) * jnp.einsum('becd,edf->becf', xin, w3)
    y = jnp.einsum('becf,efd->becd', hid, w2) * gates[..., None].astype(h.dtype)
    return jax.vmap(lambda ib, yb: jax.ops.segment_sum(yb.reshape(-1, D), ib.reshape(-1), num_segments=S))(idx, y)


def setup_inputs(seed: int = 0) -> dict:
    key = jax.random.key(seed)
    ks = jax.random.split(key, 20)
    f32 = jnp.float32
    n_a = (DEPTH + 1) // N_MIXERS
    n_b = DEPTH // N_MIXERS

    def nrm(k, shape, scale):
        return jax.random.normal(k, shape, f32) * scale

    return {
        "x": nrm(ks[0], (BATCH, SEQ, D_MODEL), 1.0),
        "mix_norm": 1.0 + nrm(ks[1], (DEPTH, D_MODEL), 0.02),
        "ffn_norm": 1.0 + nrm(ks[2], (DEPTH, D_MODEL), 0.02),
        "gm_w_in": nrm(ks[3], (n_a, D_MODEL, 2 * GMLP_WIDTH), D_MODEL ** -0.5),
        "gm_b_in": nrm(ks[4], (n_a, 2 * GMLP_WIDTH), 0.02),
        "gm_v_norm": 1.0 + nrm(ks[5], (n_a, GMLP_WIDTH), 0.02),
        "gm_w_s": nrm(ks[6], (n_a, GMLP_GROUPS, GMLP_CHUNK, GMLP_CHUNK), GMLP_CHUNK ** -0.5),
        "gm_b_s": 1.0 + nrm(ks[7], (n_a, GMLP_GROUPS, GMLP_CHUNK), 0.02),
        "gm_w_out": nrm(ks[8], (n_a, GMLP_WIDTH, D_MODEL), GMLP_WIDTH ** -0.5),
        "gm_b_out": nrm(ks[9], (n_a, D_MODEL), 0.02),
        "at_w_qkv": nrm(ks[10], (n_b, D_MODEL, QKV_WIDTH), D_MODEL ** -0.5),
        "at_q_norm": 1.0 + nrm(ks[11], (n_b, N_DIL_GROUPS, HEAD_DIM), 0.02),
        "at_k_norm": 1.0 + nrm(ks[12], (n_b, N_DIL_GROUPS, HEAD_DIM), 0.02),
        "at_w_o": nrm(ks[13], (n_b, ATTN_WIDTH, D_MODEL), ATTN_WIDTH ** -0.5),
        "moe_w_router": nrm(ks[14], (DEPTH, D_MODEL, N_EXPERTS), D_MODEL ** -0.5),
        "moe_b_router": nrm(ks[15], (DEPTH, N_EXPERTS), 0.01),
        "moe_w1": nrm(ks[16], (DEPTH, N_EXPERTS, D_MODEL, D_EXPERT), D_MODEL ** -0.5),
        "moe_w3": nrm(ks[17], (DEPTH, N_EXPERTS, D_MODEL, D_EXPERT), D_MODEL ** -0.5),
        "moe_w2": nrm(ks[18], (DEPTH, N_EXPERTS, D_EXPERT, D_MODEL), D_EXPERT ** -0.5),
    }


def reference(x, mix_norm, ffn_norm, gm_w_in, gm_b_in, gm_v_norm, gm_w_s, gm_b_s, gm_w_out, gm_b_out,
              at_w_qkv, at_q_norm, at_k_norm, at_w_o,
              moe_w_router, moe_b_router, moe_w1, moe_w3, moe_w2):
    for i in range(DEPTH):
        j = i // N_MIXERS
        h = rmsnorm(x, mix_norm[i])
        if i % N_MIXERS == 0:
            x = x + gmlp_mixer(h, gm_w_in[j], gm_b_in[j], gm_v_norm[j], gm_w_s[j], gm_b_s[j],
                               gm_w_out[j], gm_b_out[j])
        else:
            x = x + dilated_mixer(h, at_w_qkv[j], at_q_norm[j], at_k_norm[j], at_w_o[j])
        h = rmsnorm(x, ffn_norm[i])
        x = x + ec_moe(h, moe_w_router[i], moe_b_router[i], moe_w1[i], moe_w3[i], moe_w2[i])
    return x
```

```python
import contextlib
import numpy as np
import concourse.bass as bass
import concourse.mybir as mybir
from concourse.bass_utils import run_bass_kernel_spmd

F32 = mybir.dt.float32
BF16 = mybir.dt.bfloat16
I32 = mybir.dt.int32
AF = mybir.ActivationFunctionType
ALU = mybir.AluOpType
GELU = AF.Gelu_apprx_tanh

S = 4096
D = 1024
NT = S // 128
EPS = 1e-6
ENGS = ("sync", "scalar", "vector", "gpsimd", "tensor")


class Buf:
    __slots__ = ("name", "writers", "readers", "unord", "pre")

    def __init__(self, name="b"):
        self.name = name
        self.writers = []
        self.readers = []
        self.unord = False
        self.pre = []


class Op:
    __slots__ = ("eng", "fn", "deps", "is_dma", "signal", "sigval", "sem", "idx")

    def __init__(self, eng, fn, is_dma):
        self.eng = eng
        self.fn = fn
        self.deps = []
        self.is_dma = is_dma
        self.signal = False
        self.sigval = None
        self.sem = None
        self.idx = None


class Prog:
    def __init__(self, nc, stack, n_dma_sems=64):
        self.nc = nc
        self.n_dma_sems = n_dma_sems
        self.esem = {e: stack.enter_context(nc.semaphore("es_" + e)) for e in ENGS}
        self.dsem = [stack.enter_context(nc.semaphore("ds_%d" % i)) for i in range(n_dma_sems)]
        self.ecount = {e: 0 for e in ENGS}
        self.dcount = [0] * n_dma_sems
        self.seen = {e: {} for e in ENGS}
        self.bufs = []
        self.nops = 0
        self._reset_phase()

    def _reset_phase(self):
        self.ops = {e: [] for e in ENGS}
        self.dma_rr = 0
        self.dma_last = [None] * self.n_dma_sems
        self.last_compute = {e: None for e in ENGS}
        for b in self.bufs:
            b.writers = []
            b.readers = []
            b.unord = False
            b.pre = []

    def buf(self, name="b"):
        b = Buf(name)
        self.bufs.append(b)
        return b

    def _add(self, eng, fn, reads, writes, uwrites, is_dma):
        op = Op(eng, fn, is_dma)
        op.idx = self.nops
        self.nops += 1
        deps = []
        for b in reads:
            deps.extend(b.writers)
        for b in writes:
            deps.extend(b.writers)
            deps.extend(b.readers)
        for b in uwrites:
            if not (b.unord and not b.readers):
                b.pre = list(b.writers) + list(b.readers)
                b.writers = []
                b.readers = []
                b.unord = True
            deps.extend(b.pre)
        if is_dma:
            s = self.dma_rr
            self.dma_rr = (self.dma_rr + 1) % self.n_dma_sems
            op.sem = s
            prev = self.dma_last[s]
            if prev is not None:
                deps.append(prev)
            self.dma_last[s] = op
        seen = set()
        for d in deps:
            if d is op or id(d) in seen:
                continue
            seen.add(id(d))
            if (not d.is_dma) and d.eng == "tensor" and eng == "tensor" and not is_dma:
                continue
            op.deps.append(d)
            if not d.is_dma:
                d.signal = True
        for b in reads:
            b.readers.append(op)
        for b in writes:
            b.writers = [op]
            b.readers = []
            b.unord = False
        for b in uwrites:
            b.writers.append(op)
        self.ops[eng].append(op)
        if not is_dma:
            self.last_compute[eng] = op
        return op

    def op(self, eng, fn, reads=(), writes=(), uwrites=()):
        return self._add(eng, fn, reads, writes, uwrites, False)

    def dma(self, eng, fn, reads=(), writes=(), uwrites=()):
        return self._add(eng, fn, reads, writes, uwrites, True)

    def end_phase(self):
        deps = [o for o in self.last_compute.values() if o is not None]
        deps += [o for o in self.dma_last if o is not None]
        for d in deps:
            if not d.is_dma:
                d.signal = True
        for e in ENGS:
            op = Op(e, None, False)
            op.idx = self.nops
            self.nops += 1
            op.deps = list(deps)
            self.ops[e].append(op)
        for e in ENGS:
            c = self.ecount[e]
            for o in self.ops[e]:
                if (not o.is_dma) and o.signal:
                    c += 1
                    o.sigval = c
            self.ecount[e] = c
        allops = []
        for e in ENGS:
            allops.extend(self.ops[e])
        allops.sort(key=lambda o: o.idx)
        for o in allops:
            if o.is_dma:
                self.dcount[o.sem] += 16
                o.sigval = self.dcount[o.sem]

        def run(e, h):
            seen = self.seen[e]
            for o in self.ops[e]:
                for d in o.deps:
                    if d.is_dma:
                        key = ("d", d.sem)
                        s = self.dsem[d.sem]
                    else:
                        if d.eng == e and d.fn is None:
                            continue
                        key = ("e", d.eng)
                        s = self.esem[d.eng]
                    if seen.get(key, 0) >= d.sigval:
                        continue
                    seen[key] = d.sigval
                    h.wait_ge(s, d.sigval)
                if o.fn is None:
                    continue
                ins = o.fn(h)
                if o.is_dma:
                    ins.then_inc(self.dsem[o.sem], 16)
                elif o.signal:
                    ins.then_inc(self.esem[e], 1)

        with self.nc.Block() as block:
            @block.sync
            def _(h):
                run("sync", h)

            @block.scalar
            def _(h):
                run("scalar", h)

            @block.vector
            def _(h):
                run("vector", h)

            @block.gpsimd
            def _(h):
                run("gpsimd", h)

            @block.tensor
            def _(h):
                run("tensor", h)
        self._reset_phase()


class Rot:
    def __init__(self, P, tiles):
        self.tiles = tiles
        self.bufs = [P.buf() for _ in tiles]
        self.i = 0

    def next(self):
        t, b = self.tiles[self.i], self.bufs[self.i]
        self.i = (self.i + 1) % len(self.tiles)
        return t, b


class K:
    def __init__(self, nc, stack, layers=(0, 1, 2, 3), sub=("mix", "moe")):
        self.nc = nc
        self.pid = 0
        self.P = Prog(nc, stack)
        self.layers = layers
        self.sub = sub
        dt = nc.dram_tensor
        self.x_in = dt("x", [S, D], F32, kind="ExternalInput")
        self.mix_norm = dt("mix_norm", [4, D], F32, kind="ExternalInput")
        self.ffn_norm = dt("ffn_norm", [4, D], F32, kind="ExternalInput")
        self.gm_w_in = dt("gm_w_in", [2, D, 6144], F32, kind="ExternalInput")
        self.gm_b_in = dt("gm_b_in", [2, 6144], F32, kind="ExternalInput")
        self.gm_v_norm = dt("gm_v_norm", [2, 3072], F32, kind="ExternalInput")
        self.gm_w_s = dt("gm_w_s", [2, 8, 128, 128], F32, kind="ExternalInput")
        self.gm_b_s = dt("gm_b_s", [2, 8, 128], F32, kind="ExternalInput")
        self.gm_w_out = dt("gm_w_out", [2, 3072, D], F32, kind="ExternalInput")
        self.gm_b_out = dt("gm_b_out", [2, D], F32, kind="ExternalInput")
        self.at_w_qkv = dt("at_w_qkv", [2, D, 4608], F32, kind="ExternalInput")
        self.at_q_norm = dt("at_q_norm", [2, 3, 64], F32, kind="ExternalInput")
        self.at_k_norm = dt("at_k_norm", [2, 3, 64], F32, kind="ExternalInput")
        self.at_w_o = dt("at_w_o", [2, 512, D], F32, kind="ExternalInput")
        self.moe_w_router = dt("moe_w_router", [4, D, 16], F32, kind="ExternalInput")
        self.moe_b_router = dt("moe_b_router", [4, 16], F32, kind="ExternalInput")
        self.moe_w1 = dt("moe_w1", [4, 16, D, 2048], F32, kind="ExternalInput")
        self.moe_w3 = dt("moe_w3", [4, 16, D, 2048], F32, kind="ExternalInput")
        self.moe_w2 = dt("moe_w2", [4, 16, 2048, D], F32, kind="ExternalInput")
        self.out = dt("out", [S, D], F32, kind="ExternalOutput")
        self.hT_D = dt("hT_D", [NT, 128, 8, 128], BF16)
        self.sT_D = dt("sT_D", [NT, 128, 24, 128], BF16)
        self.N_D = [dt("N_D%d" % g, [S, 520], F32) for g in range(3)]
        self.hD = dt("hD", [S, 1056], BF16)
        self.cD = dt("cD", [16, S], F32)
        self.idxD = dt("idxD", [128, 64], I32)
        sb = lambda n, s, d: stack.enter_context(nc.sbuf_tensor("p%d_%s" % (self.pid, n), s, d))
        self.ident_bf = sb("ident_bf", [128, 128], BF16)
        self.ident_f = sb("ident_f", [128, 128], F32)
        self.blockones = sb("blockones", [128, 128], BF16)
        self.b_const = self.P.buf("const")
        self.pid = 0

    def consts(self):
        P = self.P
        ib, if_, bo, bc = self.ident_bf, self.ident_f, self.blockones, self.b_const
        P.op("gpsimd", lambda h: h.memset(if_[:], 1.0), writes=[bc])
        P.op("gpsimd", lambda h: h.affine_select(out=if_[:], in_=if_[:], pattern=[[-1, 128]],
                                                  compare_op=ALU.is_equal, fill=0.0, base=0,
                                                  channel_multiplier=1), reads=[bc], writes=[bc])
        P.op("vector", lambda h: h.tensor_copy(out=ib[:], in_=if_[:]), reads=[bc], writes=[bc])
        P.op("vector", lambda h: h.memset(bo[:], 0.0), reads=[bc], writes=[bc])
        P.op("vector", lambda h: h.memset(bo[0:64, 0:64], 1.0), reads=[bc], writes=[bc])
        P.op("vector", lambda h: h.memset(bo[64:128, 64:128], 1.0), reads=[bc], writes=[bc])

    def cast_load(self, dst, src, wbuf, max_bytes=4 << 20):
        P = self.P
        _, kk, n = dst.shape
        ncol = n
        while ncol > 2048:
            ncol //= 2
        kstep = max(1, min(kk, max_bytes // (128 * ncol * 4)))
        for k0 in range(0, kk, kstep):
            k1 = min(kk, k0 + kstep)
            for c0 in range(0, n, ncol):
                P.dma("gpsimd", lambda h, a=dst[:, k0:k1, c0:c0 + ncol], b=src[:, k0:k1, c0:c0 + ncol]:
                      h.dma_start(out=a, in_=b), uwrites=[wbuf])

    def bcast_load(self, dst, src_row, wbuf, eng="sync"):
        self.P.dma(eng, lambda h: h.dma_start(out=dst, in_=src_row.partition_broadcast(128)), writes=[wbuf])

    def rows_ap(self, t, ncols, start, step, nrows=128):
        return bass.AP(t, start * ncols, [[step * ncols, nrows], [1, ncols]])

    def norm_tile(self, xt, bx, gain_bc, bgain, W, hb_dtype=BF16):
        P = self.P
        junk, bjunk = W["junk"].next()
        st, bst = W["stat"].next()
        hb, bhb = W["hb"].next()
        P.op("scalar", lambda h: h.activation(out=junk[:, 0:D], in_=xt, func=AF.Square, accum_out=st[:, 0:1]),
             reads=[bx], writes=[bjunk, bst])
        P.op("scalar", lambda h: h.activation(out=st[:, 1:2], in_=st[:, 0:1], func=AF.Ln, scale=1.0 / D, bias=self.eps_t[:, 0:1]),
             reads=[bst, self.b_const], writes=[bst])
        P.op("scalar", lambda h: h.activation(out=st[:, 2:3], in_=st[:, 1:2], func=AF.Exp, scale=-0.5), reads=[bst], writes=[bst])
        P.op("vector", lambda h: h.scalar_tensor_tensor(out=hb[:], in0=xt, scalar=st[:, 2:3], in1=gain_bc[:],
                                                        op0=ALU.mult, op1=ALU.mult),
             reads=[bx, bst, bgain], writes=[bhb])
        return hb, bhb

    def transpose8(self, hb, bhb, dst, bdst, W, eng="vector"):
        P = self.P
        pT, bpT = W["pT"].next()
        for k in range(8):
            P.op("tensor", lambda h, k=k: h.transpose(out=pT[:, k, :], in_=hb[:, k * 128:(k + 1) * 128],
                                                      identity=self.ident_bf[:]),
                 reads=[bhb, self.b_const], writes=[bpT])
        if eng == "vector":
            P.op("vector", lambda h: h.tensor_copy(out=dst, in_=pT[:]), reads=[bpT], writes=[bdst])
        else:
            P.op("scalar", lambda h: h.copy(out=dst, in_=pT[:]), reads=[bpT], writes=[bdst])

    def phase(self):
        self.pid += 1
        return contextlib.ExitStack()

    def gmlp(self, i, src):
        nc, P = self.nc, self.P
        j = i // 2
        with self.phase() as st:
            sb = lambda n, s, d: st.enter_context(nc.sbuf_tensor("p%d_%s" % (self.pid, n), s, d))
            ps = lambda n, s, d: st.enter_context(nc.psum_tensor("p%d_%s" % (self.pid, n), s, d))
            wv = sb("wv", [128, 8, 3072], BF16); bwv = P.buf()
            gain_bc = sb("gain_bc", [128, D], F32); bgain = P.buf()
            bv_bc = sb("bv_bc", [128, 3072], F32); bbv = P.buf()
            wsf = sb("wsf", [128, 8, 128], F32); wsb = sb("wsb", [128, 8, 128], BF16)
            wsT = sb("wsT", [128, 8, 128], BF16); bws = P.buf()
            vgT = sb("vgT", [128, 24], F32); bvg = P.buf()
            bs_bc = sb("bs_bc", [128, 8, 128], F32); bbs = P.buf()
            self.eps_t = sb("eps_t", [128, 1], F32)
            P.op("vector", lambda h: h.memset(self.eps_t[:], EPS), reads=[self.b_const], writes=[self.b_const])
            W = {
                "junk": Rot(P, [sb("junk", [128, 3072], BF16)]),
                "stat": Rot(P, [sb("stat%d" % q, [128, 4], F32) for q in range(4)]),
                "hb": Rot(P, [sb("hb%d" % q, [128, D], BF16) for q in range(3)]),
                "pT": Rot(P, [ps("pT%d" % q, [128, 8, 128], BF16) for q in range(2)]),
            }
            xts = Rot(P, [sb("xt%d" % q, [128, D], F32) for q in range(3)])
            hTs = Rot(P, [sb("hT%d" % q, [128, 8, 128], BF16) for q in range(3)])
            pvs = Rot(P, [ps("pv%d" % q, [128, 512], F32) for q in range(3)])
            tmps = Rot(P, [sb("tmp%d" % q, [128, 512], F32) for q in range(4)])
            vs = Rot(P, [sb("v%d" % q, [128, 3072], BF16) for q in range(3)])
            vns = Rot(P, [sb("vn%d" % q, [128, 3072], BF16) for q in range(3)])
            pss = Rot(P, [ps("pss%d" % q, [128, 4, 128], F32) for q in range(3)])
            sTs = Rot(P, [sb("sT%d" % q, [128, 24, 128], BF16) for q in range(3)])
            stv = Rot(P, [sb("stv%d" % q, [128, 4], F32) for q in range(4)])
            self.bcast_load(gain_bc[:], self.mix_norm[i], bgain)
            self.bcast_load(bv_bc[:], self.gm_b_in[j, 3072:6144], bbv)
            self.bcast_load(bs_bc[:].rearrange("p g t -> p (g t)"), self.gm_b_s[j].rearrange("g t -> (g t)"), bbs)
            P.dma("sync", lambda h: h.dma_start(out=vgT[:], in_=self.gm_v_norm[j].rearrange("(c p) -> p c", p=128),
                                                allow_slow_non_contiguous=True), writes=[bvg])
            P.dma("sync", lambda h: h.dma_start(out=wsf[:], in_=self.gm_w_s[j].rearrange("g t s -> t g s")), writes=[bws])
            self.cast_load(wv[:], self.gm_w_in[j, :, 3072:6144].rearrange("(k p) f -> p k f", p=128), bwv)
            P.op("vector", lambda h: h.tensor_copy(out=wsb[:], in_=wsf[:]), reads=[bws], writes=[bws])
            pT0, bpT0 = W["pT"].next()
            for g in range(8):
                P.op("tensor", lambda h, g=g: h.transpose(out=pT0[:, g, :], in_=wsb[:, g, :], identity=self.ident_bf[:]),
                     reads=[bws, self.b_const], writes=[bpT0])
            P.op("vector", lambda h: h.tensor_copy(out=wsT[:], in_=pT0[:]), reads=[bpT0, bws], writes=[bws])
            for t in range(NT):
                xt, bx = xts.next()
                P.dma("sync", lambda h, xt=xt, t=t: h.dma_start(out=xt[:], in_=src[t * 128:(t + 1) * 128, :]), writes=[bx])
                hb, bhb = self.norm_tile(xt[:], bx, gain_bc, bgain, W)
                hT, bhT = hTs.next()
                self.transpose8(hb, bhb, hT[:], bhT, W)
                P.dma("sync", lambda h, hT=hT, t=t: h.dma_start(out=self.hT_D[t], in_=hT[:]), reads=[bhT])
                v, bv = vs.next()
                for blk in range(6):
                    pv, bpv = pvs.next()
                    for k in range(8):
                        P.op("tensor", lambda h, pv=pv, hT=hT, k=k, blk=blk: h.matmul(
                            pv[:], lhsT=hT[:, k, :], rhs=wv[:, k, blk * 512:(blk + 1) * 512], start=(k == 0), stop=(k == 7)),
                            reads=[bhT, bwv], writes=[bpv])
                    tmp, btmp = tmps.next()
                    P.op("vector", lambda h, tmp=tmp, pv=pv, blk=blk: h.tensor_tensor(
                        out=tmp[:], in0=pv[:], in1=bv_bc[:, blk * 512:(blk + 1) * 512], op=ALU.add),
                        reads=[bpv, bbv], writes=[btmp])
                    P.op("scalar", lambda h, tmp=tmp, v=v, blk=blk: h.activation(
                        out=v[:, blk * 512:(blk + 1) * 512], in_=tmp[:], func=GELU), reads=[btmp], writes=[bv])
                junk, bjunk = W["junk"].next()
                sv, bsv = stv.next()
                P.op("scalar", lambda h, junk=junk, v=v, sv=sv: h.activation(out=junk[:], in_=v[:], func=AF.Square, accum_out=sv[:, 0:1]),
                     reads=[bv], writes=[bjunk, bsv])
                P.op("scalar", lambda h, sv=sv: h.activation(out=sv[:, 1:2], in_=sv[:, 0:1], func=AF.Ln, scale=1.0 / 3072, bias=self.eps_t[:, 0:1]),
                     reads=[bsv, self.b_const], writes=[bsv])
                P.op("scalar", lambda h, sv=sv: h.activation(out=sv[:, 2:3], in_=sv[:, 1:2], func=AF.Exp, scale=-0.5), reads=[bsv], writes=[bsv])
                vn, bvn = vns.next()
                P.op("vector", lambda h, vn=vn, v=v, sv=sv: h.tensor_scalar(out=vn[:], in0=v[:], scalar1=sv[:, 2:3], scalar2=None, op0=ALU.mult),
                     reads=[bv, bsv], writes=[bvn])
                sT, bsT = sTs.next()
                for q in range(6):
                    pq, bpq = pss.next()
                    for m in range(4):
                        fc = q * 4 + m
                        P.op("tensor", lambda h, pq=pq, vn=vn, fc=fc, m=m: h.matmul(
                            pq[:, m, :], lhsT=vn[:, fc * 128:(fc + 1) * 128], rhs=wsT[:, fc // 3, :], start=True, stop=True),
                            reads=[bvn, bws], writes=[bpq])
                    for m in range(4):
                        fc = q * 4 + m
                        P.op("vector", lambda h, pq=pq, sT=sT, fc=fc, m=m: h.scalar_tensor_tensor(
                            out=sT[:, fc, :], in0=pq[:, m, :], scalar=vgT[:, fc:fc + 1], in1=bs_bc[:, fc // 3, :],
                            op0=ALU.mult, op1=ALU.add), reads=[bpq, bvg, bbs], writes=[bsT])
                P.dma("sync", lambda h, sT=sT, t=t: h.dma_start(out=self.sT_D[t], in_=sT[:]), reads=[bsT])
            P.end_phase()
        with self.phase() as st:
            sb = lambda n, s, d: st.enter_context(nc.sbuf_tensor("p%d_%s" % (self.pid, n), s, d))
            ps = lambda n, s, d: st.enter_context(nc.psum_tensor("p%d_%s" % (self.pid, n), s, d))
            wu = sb("wu", [128, 8, 3072], BF16); bwu = P.buf()
            wo = sb("wo", [128, 24, D], BF16); bwo = P.buf()
            buT = sb("buT", [128, 24], F32); bbu = P.buf()
            bout_bc = sb("bout_bc", [128, D], F32); bbo = P.buf()
            hTss = Rot(P, [sb("hTs%d" % q, [128, 2, 8, 128], BF16) for q in range(2)])
            sTss = Rot(P, [sb("sTs%d" % q, [128, 2, 24, 128], BF16) for q in range(2)])
            xss = Rot(P, [sb("xs%d" % q, [128, 2, D], F32) for q in range(2)])
            xns = Rot(P, [sb("xn%d" % q, [128, 2, D], F32) for q in range(2)])
            gTs = Rot(P, [sb("gT", [128, 24, 2, 128], BF16)])
            uts = Rot(P, [sb("ut%d" % q, [128, 2, 128], BF16) for q in range(2)])
            pus = Rot(P, [ps("pu%d" % q, [128, 2, 128], F32) for q in range(3)])
            pos = Rot(P, [ps("po%d" % q, [128, 512], F32) for q in range(2)])
            self.bcast_load(bout_bc[:], self.gm_b_out[j], bbo)
            P.dma("sync", lambda h: h.dma_start(out=buT[:], in_=self.gm_b_in[j, 0:3072].rearrange("(c p) -> p c", p=128),
                                                allow_slow_non_contiguous=True), writes=[bbu])
            self.cast_load(wu[:], self.gm_w_in[j, :, 0:3072].rearrange("(k p) f -> p k f", p=128), bwu)
            self.cast_load(wo[:], self.gm_w_out[j].rearrange("(c p) o -> p c o", p=128), bwo)
            for s in range(NT // 2):
                hTs_, bh = hTss.next()
                sTs_, bs_ = sTss.next()
                xs, bxs = xss.next()
                P.dma("sync", lambda h, a=hTs_, s=s: h.dma_start(out=a[:], in_=self.hT_D[2 * s:2 * s + 2].rearrange("t p k n -> p t k n")), writes=[bh])
                P.dma("sync", lambda h, a=sTs_, s=s: h.dma_start(out=a[:], in_=self.sT_D[2 * s:2 * s + 2].rearrange("t p c n -> p t c n")), writes=[bs_])
                P.dma("sync", lambda h, a=xs, s=s: h.dma_start(out=a[:], in_=src[s * 256:(s + 1) * 256, :].rearrange("(t p) d -> p t d", p=128)), writes=[bxs])
                for t in range(2):
                    P.op("gpsimd", lambda h, xs=xs, t=t: h.tensor_tensor(out=xs[:, t, :], in0=xs[:, t, :], in1=bout_bc[:], op=ALU.add),
                         reads=[bxs, bbo], writes=[bxs])
                gT, bgT = gTs.next()
                for fc in range(24):
                    pu, bpu = pus.next()
                    for k in range(8):
                        P.op("tensor", lambda h, pu=pu, a=hTs_, k=k, fc=fc: h.matmul(
                            pu[:], lhsT=wu[:, k, fc * 128:(fc + 1) * 128], rhs=a[:, :, k, :], start=(k == 0), stop=(k == 7)),
                            reads=[bwu, bh], writes=[bpu])
                    ut, but = uts.next()
                    P.op("scalar", lambda h, pu=pu, ut=ut, fc=fc: h.activation(out=ut[:], in_=pu[:], func=GELU, bias=buT[:, fc:fc + 1]),
                         reads=[bpu, bbu], writes=[but])
                    P.op("vector", lambda h, ut=ut, gT=gT, a=sTs_, fc=fc: h.tensor_tensor(
                        out=gT[:, fc, :, :], in0=ut[:], in1=a[:, :, fc, :], op=ALU.mult), reads=[but, bs_], writes=[bgT])
                xn, bxn = xns.next()
                for t in range(2):
                    for ob in range(2):
                        po, bpo = pos.next()
                        for fc in range(24):
                            P.op("tensor", lambda h, po=po, gT=gT, fc=fc, t=t, ob=ob: h.matmul(
                                po[:], lhsT=gT[:, fc, t, :], rhs=wo[:, fc, ob * 512:(ob + 1) * 512], start=(fc == 0), stop=(fc == 23)),
                                reads=[bgT, bwo], writes=[bpo])
                        P.op("vector", lambda h, po=po, xn=xn, xs=xs, t=t, ob=ob: h.tensor_tensor(
                            out=xn[:, t, ob * 512:(ob + 1) * 512], in0=po[:], in1=xs[:, t, ob * 512:(ob + 1) * 512], op=ALU.add),
                            reads=[bpo, bxs], writes=[bxn])
                P.dma("sync", lambda h, xn=xn, s=s: h.dma_start(out=self.out[s * 256:(s + 1) * 256, :].rearrange("(t p) d -> p t d", p=128), in_=xn[:]),
                      reads=[bxn])
            P.end_phase()

    def attn(self, i, src):
        nc, P = self.nc, self.P
        j = i // 2
        dils = (1, 4, 16)
        with self.phase() as st:
            sb = lambda n, s, d: st.enter_context(nc.sbuf_tensor("p%d_%s" % (self.pid, n), s, d))
            ps = lambda n, s, d: st.enter_context(nc.psum_tensor("p%d_%s" % (self.pid, n), s, d))
            gain_bc = sb("gain_bc", [128, D], F32); bgain = P.buf()
            self.eps_t = sb("eps_t", [128, 1], F32)
            P.op("vector", lambda h: h.memset(self.eps_t[:], EPS), reads=[self.b_const], writes=[self.b_const])
            wq = sb("wq", [128, 8, 512], BF16); wk = sb("wk", [128, 8, 512], BF16); wvv = sb("wvv", [128, 8, 512], BF16)
            bwq, bwk, bwvv = P.buf(), P.buf(), P.buf()
            gq = sb("gq", [128, 1], F32); gk = sb("gk", [128, 1], F32); bg = P.buf()
            QT = sb("QT", [128, 4, S], BF16); KT = sb("KT", [128, 4, S], BF16)
            V = sb("V", [128, NT, 8, 65], BF16)
            bQT, bKT, bV = P.buf(), P.buf(), P.buf()
            E = sb("E", [128, 3, 8, 128], BF16); bE = P.buf()
            rel = sb("rel", [128, 3, 128], F32); reli = sb("reli", [128, 3, 128], I32)
            msk = sb("msk", [128, 3, 128], F32); etmp = sb("etmp", [128, 3, 128], F32); brel = P.buf()
            W = {
                "junk": Rot(P, [sb("junk", [128, D], BF16)]),
                "stat": Rot(P, [sb("stat%d" % q, [128, 4], F32) for q in range(4)]),
                "hb": Rot(P, [sb("hb%d" % q, [128, D], BF16) for q in range(3)]),
                "pT": Rot(P, [ps("pT%d" % q, [128, 8, 128], BF16) for q in range(1)]),
            }
            xts = Rot(P, [sb("xt%d" % q, [128, D], F32) for q in range(3)])
            hTbs = Rot(P, [sb("hTb%d" % q, [128, 8, 512], BF16) for q in range(2)])
            pqs = Rot(P, [ps("pq%d" % q, [128, 512], F32) for q in range(2)])
            pns = Rot(P, [ps("pn%d" % q, [128, 512], F32) for q in range(1)])
            sqs = Rot(P, [sb("sq%d" % q, [128, 512], BF16) for q in range(2)])
            stds = Rot(P, [sb("std%d" % q, [128, 512], F32) for q in range(2)])
            pSs = Rot(P, [ps("pS%d" % q, [128, 4, 128], F32) for q in range(2)])
            pOs = [ps("pO%d" % q, [128, 4, 65], F32) for q in range(2)]
            bpOs = [P.buf(), P.buf()]
            exs = Rot(P, [sb("ex%d" % q, [128, 4, 128], BF16) for q in range(2)])
            pTs = Rot(P, [sb("pTt%d" % q, [128, 4, 128], BF16) for q in range(8)])
            Obs = Rot(P, [sb("Ob%d" % q, [128, 8, 65], F32) for q in range(2)])
            self.bcast_load(gain_bc[:], self.mix_norm[i], bgain)
            P.op("gpsimd", lambda h: h.iota(reli[:], pattern=[[128, 3], [-1, 128]], base=-128, channel_multiplier=1), writes=[brel])
            P.op("vector", lambda h: h.tensor_copy(out=rel[:], in_=reli[:]), reads=[brel], writes=[brel])
            P.op("scalar", lambda h: h.activation(out=rel[:], in_=rel[:], func=AF.Abs), reads=[brel], writes=[brel])
            P.op("vector", lambda h: h.tensor_single_scalar(out=msk[:], in_=rel[:], scalar=64.5, op=ALU.is_le), reads=[brel], writes=[brel])
            P.op("gpsimd", lambda h: h.memset(V[:, :, :, 64:65], 1.0), writes=[bV])
            for g in range(3):
                d = dils[g]
                L = S // d
                nb = L // 128
                wsrc = self.at_w_qkv[j].rearrange("(k p) f -> p k f", p=128)
                self.cast_load(wq[:], wsrc[:, :, g * 512:(g + 1) * 512], bwq)
                self.cast_load(wk[:], wsrc[:, :, 1536 + g * 512:1536 + (g + 1) * 512], bwk)
                self.cast_load(wvv[:], wsrc[:, :, 3072 + g * 512:3072 + (g + 1) * 512], bwvv)
                for half in range(2):
                    P.dma("sync", lambda h, half=half, g=g: h.dma_start(out=gq[half * 64:(half + 1) * 64, :], in_=self.at_q_norm[j, g].rearrange("(p o) -> p o", o=1)), uwrites=[bg])
                    P.dma("sync", lambda h, half=half, g=g: h.dma_start(out=gk[half * 64:(half + 1) * 64, :], in_=self.at_k_norm[j, g].rearrange("(p o) -> p o", o=1)), uwrites=[bg])
                P.op("vector", lambda h: h.tensor_scalar(out=gq[:], in0=gq[:], scalar1=0.125, scalar2=None, op0=ALU.mult), reads=[bg], writes=[bg])
                for ei in range(8):
                    hd = 2 * (ei % 4) + ei // 4
                    slope = 2.0 ** (-(hd + 1))
                    P.op("scalar", lambda h, slope=slope, d=d: h.activation(out=etmp[:], in_=rel[:], func=AF.Exp, scale=-slope * d), reads=[brel], writes=[brel])
                    P.op("vector", lambda h, ei=ei: h.tensor_tensor(out=E[:, :, ei, :], in0=etmp[:], in1=msk[:], op=ALU.mult), reads=[brel], writes=[bE])
                for pb in range(8):
                    hTb, bhTb = hTbs.next()
                    for q in range(4):
                        pt = pb * 4 + q
                        r, b = pt // nb, pt % nb
                        xt, bx = xts.next()
                        P.dma("sync", lambda h, xt=xt, r=r, b=b, d=d: h.dma_start(out=xt[:], in_=self.rows_ap(src, D, r + b * 128 * d, d)), writes=[bx])
                        hb, bhb = self.norm_tile(xt[:], bx, gain_bc, bgain, W)
                        self.transpose8(hb, bhb, hTb[:, :, q * 128:(q + 1) * 128], bhTb, W)
                    for (wt, bwt, gt, dstT, bdst) in ((wq, bwq, gq, QT, bQT), (wk, bwk, gk, KT, bKT)):
                        for c in range(4):
                            pq, bpq = pqs.next()
                            for k in range(8):
                                P.op("tensor", lambda h, pq=pq, wt=wt, hTb=hTb, k=k, c=c: h.matmul(
                                    pq[:], lhsT=wt[:, k, c * 128:(c + 1) * 128], rhs=hTb[:, k, :], start=(k == 0), stop=(k == 7)),
                                    reads=[bwt, bhTb], writes=[bpq])
                            sq, bsq = sqs.next()
                            P.op("scalar", lambda h, sq=sq, pq=pq: h.activation(out=sq[:], in_=pq[:], func=AF.Square), reads=[bpq], writes=[bsq])
                            pn, bpn = pns.next()
                            P.op("tensor", lambda h, pn=pn, sq=sq: h.matmul(pn[:], lhsT=self.blockones[:], rhs=sq[:], start=True, stop=True),
                                 reads=[bsq, self.b_const], writes=[bpn])
                            sd, bsd = stds.next()
                            P.op("scalar", lambda h, sd=sd, pn=pn: h.activation(out=sd[:], in_=pn[:], func=AF.Ln, scale=1.0 / 64, bias=self.eps_t[:, 0:1]),
                                 reads=[bpn, self.b_const], writes=[bsd])
                            P.op("scalar", lambda h, sd=sd: h.activation(out=sd[:], in_=sd[:], func=AF.Exp, scale=-0.5), reads=[bsd], writes=[bsd])
                            P.op("vector", lambda h, sd=sd, pq=pq, gt=gt, dstT=dstT, c=c, pb=pb: h.scalar_tensor_tensor(
                                out=dstT[:, c, pb * 512:(pb + 1) * 512], in0=pq[:], scalar=gt[:, 0:1], in1=sd[:], op0=ALU.mult, op1=ALU.mult),
                                reads=[bpq, bsd, bg], uwrites=[bdst])
                    for q in range(4):
                        pt = pb * 4 + q
                        pq, bpq = pqs.next()
                        for k in range(8):
                            P.op("tensor", lambda h, pq=pq, hTb=hTb, k=k, q=q: h.matmul(
                                pq[:], lhsT=hTb[:, k, q * 128:(q + 1) * 128], rhs=wvv[:, k, :], start=(k == 0), stop=(k == 7)),
                                reads=[bwvv, bhTb], writes=[bpq])
                        P.op("scalar", lambda h, pq=pq, pt=pt: h.copy(out=V[:, pt, :, 0:64], in_=pq[:].rearrange("p (a b) -> p a b", b=64)),
                             reads=[bpq], uwrites=[bV])
                for qb in range(NT):
                    r, b = qb // nb, qb % nb
                    offs = [o for o in (-1, 0, 1) if 0 <= b + o < nb]
                    pTl = {}
                    for hh in range(2):
                        for o in offs:
                            kt = qb + o
                            pS, bpS = pSs.next()
                            for m in range(4):
                                hd = 2 * m + hh
                                c, hp = hd // 2, hd % 2
                                P.op("tensor", lambda h, pS=pS, m=m, c=c, hp=hp, kt=kt, qb=qb: h.matmul(
                                    pS[:, m, :], lhsT=KT[hp * 64:(hp + 1) * 64, c, kt * 128:(kt + 1) * 128],
                                    rhs=QT[hp * 64:(hp + 1) * 64, c, qb * 128:(qb + 1) * 128], start=True, stop=True),
                                    reads=[bKT, bQT], writes=[bpS])
                            ex, bex = exs.next()
                            P.op("scalar", lambda h, ex=ex, pS=pS: h.activation(out=ex[:], in_=pS[:], func=AF.Exp), reads=[bpS], writes=[bex])
                            pT_, bpT_ = pTs.next()
                            P.op("vector", lambda h, ex=ex, pT_=pT_, o=o, hh=hh: h.tensor_tensor(
                                out=pT_[:], in0=ex[:], in1=E[:, o + 1, hh * 4:(hh + 1) * 4, :], op=ALU.mult), reads=[bex, bE], writes=[bpT_])
                            pTl[(hh, o)] = (pT_, bpT_)
                    Ob, bOb = Obs.next()
                    for hh in range(2):
                        pO, bpO = pOs[hh], bpOs[hh]
                        for m in range(4):
                            hd = 2 * m + hh
                            for oi, o in enumerate(offs):
                                kt = qb + o
                                pT_, bpT_ = pTl[(hh, o)]
                                P.op("tensor", lambda h, pO=pO, pT_=pT_, m=m, kt=kt, hd=hd, oi=oi, n=len(offs): h.matmul(
                                    pO[:, m, :], lhsT=pT_[:, m, :], rhs=V[:, kt, hd, :], start=(oi == 0), stop=(oi == n - 1)),
                                    reads=[bpT_, bV], writes=[bpO])
                        P.op("vector" if hh == 0 else "scalar",
                             (lambda h, Ob=Ob, pO=pO, hh=hh: h.tensor_copy(out=Ob[:].rearrange("p (m b) c -> p m b c", b=2)[:, :, hh, :], in_=pO[:])) if hh == 0 else
                             (lambda h, Ob=Ob, pO=pO, hh=hh: h.copy(out=Ob[:].rearrange("p (m b) c -> p m b c", b=2)[:, :, hh, :], in_=pO[:])),
                             reads=[bpO], uwrites=[bOb])
                    P.dma("sync", lambda h, Ob=Ob, r=r, b=b, d=d, g=g: h.dma_start(
                        out=self.rows_ap(self.N_D[g], 520, r + b * 128 * d, d), in_=Ob[:].rearrange("p a b -> p (a b)")), reads=[bOb])
            P.end_phase()
        with self.phase() as st:
            sb = lambda n, s, d: st.enter_context(nc.sbuf_tensor("p%d_%s" % (self.pid, n), s, d))
            ps = lambda n, s, d: st.enter_context(nc.psum_tensor("p%d_%s" % (self.pid, n), s, d))
            wo = sb("wo", [128, 4, D], BF16); bwo = P.buf()
            self.cast_load(wo[:], self.at_w_o[j].rearrange("(c p) o -> p c o", p=128), bwo)
            Ns = [Rot(P, [sb("N%d_%d" % (g, q), [128, 8, 65], F32) for q in range(4)]) for g in range(3)]
            rds = Rot(P, [sb("rd%d" % q, [128, 8], F32) for q in range(4)])
            obs = Rot(P, [sb("ob%d" % q, [128, 8, 64], BF16) for q in range(4)])
            pT2 = Rot(P, [ps("pT2_%d" % q, [128, 4, 128], BF16) for q in range(2)])
            oTs = Rot(P, [sb("oT%d" % q, [128, 4, 128], BF16) for q in range(4)])
            xts = Rot(P, [sb("xt%d" % q, [128, D], F32) for q in range(4)])
            xns = Rot(P, [sb("xn%d" % q, [128, D], F32) for q in range(4)])
            pos = Rot(P, [ps("po%d" % q, [128, 512], F32) for q in range(4)])
            for t in range(NT):
                tl = []
                for g in range(3):
                    n_, bn = Ns[g].next()
                    P.dma("sync", lambda h, n_=n_, g=g, t=t: h.dma_start(out=n_[:].rearrange("p a b -> p (a b)"), in_=self.N_D[g][t * 128:(t + 1) * 128, :]), writes=[bn])
                    tl.append((n_, bn))
                xt, bx = xts.next()
                P.dma("sync", lambda h, xt=xt, t=t: h.dma_start(out=xt[:], in_=src[t * 128:(t + 1) * 128, :]), writes=[bx])
                n0, bn0 = tl[0]
                P.op("vector", lambda h, n0=n0, n1=tl[1][0]: h.tensor_tensor(out=n0[:], in0=n0[:], in1=n1[:], op=ALU.add), reads=[tl[1][1], bn0], writes=[bn0])
                P.op("vector", lambda h, n0=n0, n2=tl[2][0]: h.tensor_tensor(out=n0[:], in0=n0[:], in1=n2[:], op=ALU.add), reads=[tl[2][1], bn0], writes=[bn0])
                rd, brd = rds.next()
                P.op("vector", lambda h, rd=rd, n0=n0: h.reciprocal(out=rd[:], in_=n0[:, :, 64]), reads=[bn0], writes=[brd])
                ob, bob = obs.next()
                P.op("vector", lambda h, ob=ob, n0=n0, rd=rd: h.tensor_tensor(
                    out=ob[:], in0=n0[:, :, 0:64], in1=rd[:].unsqueeze(2).to_broadcast([128, 8, 64]), op=ALU.mult),
                    reads=[bn0, brd], writes=[bob])
                pt_, bpt = pT2.next()
                for c in range(4):
                    P.op("tensor", lambda h, pt_=pt_, ob=ob, c=c: h.transpose(
                        out=pt_[:, c, :], in_=ob[:, 2 * c:2 * c + 2, :].rearrange("p a b -> p (a b)"), identity=self.ident_bf[:]),
                        reads=[bob, self.b_const], writes=[bpt])
                oT, boT = oTs.next()
                P.op("scalar", lambda h, oT=oT, pt_=pt_: h.copy(out=oT[:], in_=pt_[:]), reads=[bpt], writes=[boT])
                xn, bxn = xns.next()
                for obk in range(2):
                    po, bpo = pos.next()
                    for c in range(4):
                        P.op("tensor", lambda h, po=po, oT=oT, c=c, obk=obk: h.matmul(
                            po[:], lhsT=oT[:, c, :], rhs=wo[:, c, obk * 512:(obk + 1) * 512], start=(c == 0), stop=(c == 3)),
                            reads=[boT, bwo], writes=[bpo])
                    P.op("vector", lambda h, po=po, xn=xn, xt=xt, obk=obk: h.tensor_tensor(
                        out=xn[:, obk * 512:(obk + 1) * 512], in0=po[:], in1=xt[:, obk * 512:(obk + 1) * 512], op=ALU.add),
                        reads=[bpo, bx], uwrites=[bxn])
                P.dma("sync", lambda h, xn=xn, t=t: h.dma_start(out=self.out[t * 128:(t + 1) * 128, :], in_=xn[:]), reads=[bxn])
            P.end_phase()

    def moe(self, i):
        nc, P = self.nc, self.P
        src = self.out
        with self.phase() as st:
            sb = lambda n, s, d: st.enter_context(nc.sbuf_tensor("p%d_%s" % (self.pid, n), s, d))
            ps = lambda n, s, d: st.enter_context(nc.psum_tensor("p%d_%s" % (self.pid, n), s, d))
            gain_bc = sb("gain_bc", [128, D], F32); bgain = P.buf()
            self.eps_t = sb("eps_t", [128, 1], F32)
            P.op("vector", lambda h: h.memset(self.eps_t[:], EPS), reads=[self.b_const], writes=[self.b_const])
            wr = sb("wr", [128, 8, 16], F32); bwr = P.buf()
            br_bc = sb("br_bc", [128, 16], F32); bbr = P.buf()
            affT = sb("affT", [16, S], F32); baffT = P.buf()
            cjunk = sb("cjunk", [128, S], BF16); bcj = P.buf()
            ajunk = sb("ajunk", [128, S], BF16); baj = P.buf()
            lo = sb("lo", [16, 4], F32); blo = P.buf()
            jvi = sb("jvi", [128, 4], I32); jv = sb("jv", [128, 4], F32); jvh = sb("jvh", [128, 4], F32); bjv = P.buf()
            idxf = sb("idxf", [128, 64], F32); idxi = sb("idxi", [128, 64], I32); bidx = P.buf()
            xts = Rot(P, [sb("xt%d" % q, [128, D], F32) for q in range(4)])
            junks = Rot(P, [sb("junk%d" % q, [128, D], BF16) for q in range(2)])
            stats = Rot(P, [sb("stat%d" % q, [128, 8], F32) for q in range(4)])
            hfs = Rot(P, [sb("hf%d" % q, [128, D], F32) for q in range(4)])
            hbs = Rot(P, [sb("hbx%d" % q, [128, 1056], BF16) for q in range(4)])
            pTfs = Rot(P, [ps("pTf%d" % q, [128, 4, 128], F32) for q in range(4)])
            hT32s = Rot(P, [sb("hT32_%d" % q, [128, 8, 128], F32) for q in range(3)])
            prs = Rot(P, [ps("pr%d" % q, [128, 16], F32) for q in range(2)])
            lgs = Rot(P, [sb("lg%d" % q, [128, 16], F32) for q in range(4)])
            pats = Rot(P, [ps("pat%d" % q, [16, 128], F32) for q in range(2)])
            cbcs = Rot(P, [sb("cbc%d" % q, [128, S], F32) for q in range(2)])
            self.bcast_load(gain_bc[:], self.ffn_norm[i], bgain)
            self.bcast_load(br_bc[:], self.moe_b_router[i], bbr)
            P.dma("sync", lambda h: h.dma_start(out=wr[:], in_=self.moe_w_router[i].rearrange("(k p) e -> p k e", p=128)), writes=[bwr])
            for t in range(NT):
                xt, bx = xts.next()
                P.dma("sync", lambda h, xt=xt, t=t: h.dma_start(out=xt[:], in_=src[t * 128:(t + 1) * 128, :]), writes=[bx])
                junk, bjunk = junks.next()
                stt, bst = stats.next()
                hf, bhf = hfs.next()
                P.op("scalar", lambda h, junk=junk, xt=xt, stt=stt: h.activation(out=junk[:], in_=xt[:], func=AF.Square, accum_out=stt[:, 0:1]),
                     reads=[bx], writes=[bjunk, bst])
                P.op("scalar", lambda h, stt=stt: h.activation(out=stt[:, 1:2], in_=stt[:, 0:1], func=AF.Ln, scale=1.0 / D, bias=self.eps_t[:, 0:1]),
                     reads=[bst, self.b_const], writes=[bst])
                P.op("scalar", lambda h, stt=stt: h.activation(out=stt[:, 2:3], in_=stt[:, 1:2], func=AF.Exp, scale=-0.5), reads=[bst], writes=[bst])
                P.op("vector", lambda h, hf=hf, xt=xt, stt=stt: h.scalar_tensor_tensor(out=hf[:], in0=xt[:], scalar=stt[:, 2:3], in1=gain_bc[:], op0=ALU.mult, op1=ALU.mult),
                     reads=[bx, bst, bgain], writes=[bhf])
                hb, bhb = hbs.next()
                P.op("gpsimd", lambda h, hb=hb, hf=hf: h.tensor_copy(out=hb[:, 0:D], in_=hf[:]), reads=[bhf], uwrites=[bhb])
                hT32, bhT32 = hT32s.next()
                for half in range(2):
                    pTf, bpTf = pTfs.next()
                    for k in range(4):
                        kk = half * 4 + k
                        P.op("tensor", lambda h, pTf=pTf, hf=hf, k=k, kk=kk: h.transpose(out=pTf[:, k, :], in_=hf[:, kk * 128:(kk + 1) * 128], identity=self.ident_f[:]),
                             reads=[bhf, self.b_const], writes=[bpTf])
                    if half == 0:
                        P.op("vector", lambda h, hT32=hT32, pTf=pTf: h.tensor_copy(out=hT32[:, 0:4, :], in_=pTf[:]), reads=[bpTf], uwrites=[bhT32])
                    else:
                        P.op("scalar", lambda h, hT32=hT32, pTf=pTf: h.copy(out=hT32[:, 4:8, :], in_=pTf[:]), reads=[bpTf], uwrites=[bhT32])
                pr, bpr = prs.next()
                for k in range(8):
                    P.op("tensor", lambda h, pr=pr, hT32=hT32, k=k: h.matmul(pr[:], lhsT=hT32[:, k, :], rhs=wr[:, k, :], start=(k == 0), stop=(k == 7)),
                         reads=[bhT32, bwr], writes=[bpr])
                lg, blg = lgs.next()
                P.op("vector", lambda h, lg=lg, pr=pr: h.tensor_tensor(out=lg[:], in0=pr[:], in1=br_bc[:], op=ALU.add), reads=[bpr, bbr], writes=[blg])
                P.op("vector", lambda h, lg=lg, stt=stt: h.reduce_max(out=stt[:, 3:4], in_=lg[:], axis=mybir.AxisListType.X), reads=[blg, bst], writes=[bst])
                P.op("vector", lambda h, stt=stt: h.tensor_scalar(out=stt[:, 4:5], in0=stt[:, 3:4], scalar1=-1.0, scalar2=None, op0=ALU.mult), reads=[bst], writes=[bst])
                P.op("scalar", lambda h, lg=lg, stt=stt: h.activation(out=lg[:], in_=lg[:], func=AF.Exp, bias=stt[:, 4:5], accum_out=stt[:, 5:6]),
                     reads=[blg, bst], writes=[blg, bst])
                P.op("vector", lambda h, stt=stt: h.reciprocal(out=stt[:, 6:7], in_=stt[:, 5:6]), reads=[bst], writes=[bst])
                P.op("vector", lambda h, lg=lg, stt=stt: h.tensor_scalar(out=lg[:], in0=lg[:], scalar1=stt[:, 6:7], scalar2=None, op0=ALU.mult), reads=[blg, bst], writes=[blg])
                P.op("gpsimd", lambda h, hb=hb, lg=lg: h.tensor_copy(out=hb[:, D:1056].bitcast(F32), in_=lg[:]), reads=[blg], uwrites=[bhb])
                P.dma("sync", lambda h, hb=hb, t=t: h.dma_start(out=self.hD[t * 128:(t + 1) * 128, :], in_=hb[:]), reads=[bhb])
                pat, bpat = pats.next()
                P.op("tensor", lambda h, pat=pat, lg=lg: h.transpose(out=pat[:], in_=lg[:], identity=self.ident_f[:]), reads=[blg, self.b_const], writes=[bpat])
                P.op("scalar", lambda h, pat=pat, t=t: h.copy(out=affT[:, t * 128:(t + 1) * 128], in_=pat[:]), reads=[bpat], uwrites=[baffT])
            P.op("vector", lambda h: h.memset(lo[:], 0.0), writes=[blo])
            for it in range(30):
                hstep = 2.0 ** (-(it + 1))
                P.op("vector", lambda h, hstep=hstep: h.tensor_scalar(out=lo[:, 1:2], in0=lo[:, 0:1], scalar1=hstep, scalar2=None, op0=ALU.add), reads=[blo], writes=[blo])
                P.op("vector", lambda h: h.tensor_scalar(out=cjunk[0:16, :], in0=affT[:], scalar1=lo[:, 1:2], scalar2=0.0, op0=ALU.is_ge, op1=ALU.add, accum_out=lo[:, 2:3]),
                     reads=[baffT, blo], writes=[bcj, blo])
                P.op("vector", lambda h, hstep=hstep: h.tensor_scalar(out=lo[:, 3:4], in0=lo[:, 2:3], scalar1=511.5, scalar2=hstep, op0=ALU.is_ge, op1=ALU.mult), reads=[blo], writes=[blo])
                P.op("vector", lambda h: h.tensor_tensor(out=lo[:, 0:1], in0=lo[:, 0:1], in1=lo[:, 3:4], op=ALU.add), reads=[blo], writes=[blo])
            P.op("vector", lambda h: h.tensor_scalar(out=affT[:], in0=affT[:], scalar1=lo[:, 0:1], scalar2=None, op0=ALU.is_ge), reads=[baffT, blo], writes=[baffT])
            P.op("vector", lambda h: h.tensor_tensor_scan(out=affT[:], data0=affT[:], data1=affT[:], initial=0.0, op0=ALU.add, op1=ALU.bypass), reads=[baffT], writes=[baffT])
            bcD = P.buf()
            P.dma("sync", lambda h: h.dma_start(out=self.cD.ap(), in_=affT[:]), reads=[baffT], writes=[bcD])
            P.op("gpsimd", lambda h: h.iota(jvi[:], pattern=[[128, 4]], base=0, channel_multiplier=1), writes=[bjv])
            P.op("vector", lambda h: h.tensor_copy(out=jv[:], in_=jvi[:]), reads=[bjv], writes=[bjv])
            P.op("vector", lambda h: h.tensor_scalar(out=jvh[:], in0=jv[:], scalar1=0.5, scalar2=None, op0=ALU.add), reads=[bjv], writes=[bjv])
            for e in range(16):
                cbc, bcbc = cbcs.next()
                P.dma("sync", lambda h, cbc=cbc, e=e: h.dma_start(out=cbc[:], in_=self.cD[e].partition_broadcast(128)), reads=[bcD], writes=[bcbc])
                for J in range(4):
                    col = e * 4 + J
                    if J < 2:
                        P.op("vector", lambda h, cbc=cbc, J=J, col=col: h.tensor_scalar(
                            out=cjunk[:], in0=cbc[:], scalar1=jv[:, J:J + 1], scalar2=0.0, op0=ALU.is_le, op1=ALU.add, accum_out=idxf[:, col:col + 1]),
                            reads=[bcbc, bjv], writes=[bcj], uwrites=[bidx])
                    else:
                        P.op("scalar", lambda h, cbc=cbc, J=J, col=col: h.activation(
                            out=ajunk[:], in_=cbc[:], func=AF.Sign, scale=-1.0, bias=jvh[:, J:J + 1], accum_out=idxf[:, col:col + 1]),
                            reads=[bcbc, bjv], writes=[baj], uwrites=[bidx])
            iv = idxf[:].rearrange("p (e j) -> p e j", j=4)
            P.op("vector", lambda h: h.tensor_scalar(out=iv[:, :, 2:4], in0=iv[:, :, 2:4], scalar1=0.5, scalar2=2048.0, op0=ALU.mult, op1=ALU.add), reads=[bidx], writes=[bidx])
            P.op("vector", lambda h: h.tensor_copy(out=idxi[:], in_=idxf[:]), reads=[bidx], writes=[bidx])
            P.dma("sync", lambda h: h.dma_start(out=self.idxD.ap(), in_=idxi[:]), reads=[bidx])
            P.end_phase()
        with self.phase() as st:
            sb = lambda n, s, d: st.enter_context(nc.sbuf_tensor("p%d_%s" % (self.pid, n), s, d))
            ps = lambda n, s, d: st.enter_context(nc.psum_tensor("p%d_%s" % (self.pid, n), s, d))
            NS = 8
            ring = [sb("ring%d" % q, [128, 8, 1024], BF16) for q in range(NS)]
            bring = [P.buf() for _ in range(NS)]
            idx = sb("idx", [128, 64], I32); bidx = P.buf()
            xins = Rot(P, [sb("xin%d" % q, [128, 4, 1056], BF16) for q in range(2)])
            xinTs = Rot(P, [sb("xinT%d" % q, [128, 8, 512], BF16) for q in range(1)])
            hidTs = Rot(P, [sb("hidT", [128, 16, 512], BF16)])
            sls = Rot(P, [sb("sl%d" % q, [128, 512], BF16) for q in range(2)])
            ybs = Rot(P, [sb("yb%d" % q, [128, D], F32) for q in range(2)])
            pTs_ = Rot(P, [ps("pTx%d" % q, [128, 8, 128], BF16) for q in range(2)])
            pAs = Rot(P, [ps("pA%d" % q, [128, 512], F32) for q in range(2)])
            pBs = Rot(P, [ps("pB%d" % q, [128, 512], F32) for q in range(2)])
            pYs = Rot(P, [ps("pY%d" % q, [128, 512], F32) for q in range(2)])
            bout = P.buf()
            regbox = {}
            P.dma("sync", lambda h: h.dma_start(out=idx[:], in_=self.idxD.ap()), writes=[bidx])

            def unit_src(e, u):
                if u < 4:
                    w = self.moe_w1 if u % 2 == 0 else self.moe_w3
                    hf = u // 2
                    return w[i, e, :, hf * 1024:(hf + 1) * 1024].rearrange("(k p) f -> p k f", p=128)
                hf = u - 4
                return self.moe_w2[i, e, hf * 1024:(hf + 1) * 1024, :].rearrange("(c p) o -> p c o", p=128)

            def load_unit(e, u):
                s = (6 * e + u) % NS
                self.cast_load(ring[s][:], unit_src(e, u), bring[s])

            def gather(e):
                xin, bxin = xins.next()
                for J in range(4):
                    col = e * 4 + J
                    P.dma("gpsimd", lambda h, xin=xin, J=J, col=col: h.indirect_dma_start(
                        out=xin[:, J, :], out_offset=None, in_=self.hD.ap(),
                        in_offset=bass.IndirectOffsetOnAxis(ap=idx[:, col:col + 1], axis=0)), reads=[bidx], uwrites=[bxin])
                return xin, bxin

            nxt = gather(0)
            for u in range(6):
                load_unit(0, u)
            for e in range(16):
                xin, bxin = nxt
                if e + 1 < 16:
                    nxt = gather(e + 1)
                xinT, bxT = xinTs.next()
                for J in range(4):
                    pT_, bpT_ = pTs_.next()
                    for k in range(8):
                        P.op("tensor", lambda h, pT_=pT_, xin=xin, J=J, k=k: h.transpose(out=pT_[:, k, :], in_=xin[:, J, k * 128:(k + 1) * 128], identity=self.ident_bf[:]),
                             reads=[bxin, self.b_const], writes=[bpT_])
                    if J % 2 == 0:
                        P.op("vector", lambda h, pT_=pT_, xinT=xinT, J=J: h.tensor_copy(out=xinT[:, :, J * 128:(J + 1) * 128], in_=pT_[:]), reads=[bpT_], uwrites=[bxT])
                    else:
                        P.op("scalar", lambda h, pT_=pT_, xinT=xinT, J=J: h.copy(out=xinT[:, :, J * 128:(J + 1) * 128], in_=pT_[:]), reads=[bpT_], uwrites=[bxT])
                hidT, bhid = hidTs.next()
                for hf in range(2):
                    s1 = (6 * e + 2 * hf) % NS
                    s3 = (6 * e + 2 * hf + 1) % NS
                    for fcl in range(8):
                        fc = hf * 8 + fcl
                        pA, bpA = pAs.next()
                        pB, bpB = pBs.next()
                        for k in range(8):
                            P.op("tensor", lambda h, pA=pA, s1=s1, k=k, fcl=fcl, xinT=xinT: h.matmul(
                                pA[:], lhsT=ring[s1][:, k, fcl * 128:(fcl + 1) * 128], rhs=xinT[:, k, :], start=(k == 0), stop=(k == 7)),
                                reads=[bring[s1], bxT], writes=[bpA])
                        for k in range(8):
                            P.op("tensor", lambda h, pB=pB, s3=s3, k=k, fcl=fcl, xinT=xinT: h.matmul(
                                pB[:], lhsT=ring[s3][:, k, fcl * 128:(fcl + 1) * 128], rhs=xinT[:, k, :], start=(k == 0), stop=(k == 7)),
                                reads=[bring[s3], bxT], writes=[bpB])
                        sl, bsl = sls.next()
                        P.op("scalar", lambda h, sl=sl, pA=pA: h.activation(out=sl[:], in_=pA[:], func=AF.Silu), reads=[bpA], writes=[bsl])
                        P.op("vector", lambda h, sl=sl, pB=pB, hidT=hidT, fc=fc: h.tensor_tensor(out=hidT[:, fc, :], in0=pB[:], in1=sl[:], op=ALU.mult),
                             reads=[bpB, bsl], uwrites=[bhid])
                    if e + 1 < 16:
                        if hf == 0:
                            load_unit(e + 1, 0)
                            load_unit(e + 1, 1)
                            load_unit(e + 1, 2)
                            load_unit(e + 1, 3)
                        else:
                            load_unit(e + 1, 4)
                            load_unit(e + 1, 5)
                sA = (6 * e + 4) % NS
                sB = (6 * e + 5) % NS
                for J in range(4):
                    yb, byb = ybs.next()
                    for ob in range(2):
                        pY, bpY = pYs.next()
                        for fc in range(16):
                            sw = sA if fc < 8 else sB
                            P.op("tensor", lambda h, pY=pY, hidT=hidT, fc=fc, J=J, ob=ob, sw=sw: h.matmul(
                                pY[:], lhsT=hidT[:, fc, J * 128:(J + 1) * 128], rhs=ring[sw][:, fc % 8, ob * 512:(ob + 1) * 512], start=(fc == 0), stop=(fc == 15)),
                                reads=[bhid, bring[sw]], writes=[bpY])
                        gate = xin[:, J, D:1056].bitcast(F32)[:, e:e + 1]
                        if ob == 0:
                            P.op("vector", lambda h, yb=yb, pY=pY, gate=gate, ob=ob: h.tensor_scalar(out=yb[:, ob * 512:(ob + 1) * 512], in0=pY[:], scalar1=gate, scalar2=None, op0=ALU.mult),
                                 reads=[bpY, bxin], uwrites=[byb])
                        else:
                            P.op("scalar", lambda h, yb=yb, pY=pY, gate=gate, ob=ob: h.activation(out=yb[:, ob * 512:(ob + 1) * 512], in_=pY[:], func=AF.Copy, scale=gate),
                                 reads=[bpY, bxin], uwrites=[byb])
                    col = e * 4 + J
                    def scat(h, yb=yb, col=col):
                        if "r" not in regbox:
                            regbox["r"] = h.to_reg(S - 1)
                        return h.indirect_dma_start(
                            out=self.out.ap(), out_offset=bass.IndirectOffsetOnAxis(ap=idx[:, col:col + 1], axis=0), in_=yb[:], in_offset=None,
                            bounds_check=regbox["r"], oob_is_err=True, compute_op=ALU.add)
                    P.dma("gpsimd", scat, reads=[byb, bidx], writes=[bout])
            P.end_phase()

    def build(self):
        P = self.P
        self.consts()
        first = True
        for i in self.layers:
            if "mix" in self.sub:
                src = self.x_in if first else self.out
                if i % 2 == 0:
                    self.gmlp(i, src)
                else:
                    self.attn(i, src)
                first = False
            if "moe" in self.sub:
                if first:
                    with self.phase() as st:
                        t = st.enter_context(self.nc.sbuf_tensor("cp%d" % self.pid, [128, 8, D], F32))
                        b = P.buf()
                        for q in range(4):
                            P.dma("sync", lambda h, q=q: h.dma_start(out=t[:], in_=self.x_in[q * 1024:(q + 1) * 1024, :].rearrange("(a p) d -> p a d", p=128)), writes=[b])
                            P.dma("sync", lambda h, q=q: h.dma_start(out=self.out[q * 1024:(q + 1) * 1024, :].rearrange("(a p) d -> p a d", p=128), in_=t[:]), reads=[b])
                        P.end_phase()
                    first = False
                self.moe(i)


_WNAMES = ["mix_norm", "ffn_norm", "gm_w_in", "gm_b_in", "gm_v_norm", "gm_w_s", "gm_b_s", "gm_w_out", "gm_b_out",
           "at_w_qkv", "at_q_norm", "at_k_norm", "at_w_o", "moe_w_router", "moe_b_router", "moe_w1", "moe_w3", "moe_w2"]


def build_nc(layers=(0, 1, 2, 3), sub=("mix", "moe")):
    nc = bass.Bass("TRN2", target_bir_lowering=False)
    with contextlib.ExitStack() as stack:
        k = K(nc, stack, layers, sub)
        k.build()
    return nc


def kernel(**inputs):
    x = np.ascontiguousarray(inputs["x"], dtype=np.float32)
    B = x.shape[0]
    w = {n: np.ascontiguousarray(inputs[n], dtype=np.float32) for n in _WNAMES}
    nc = build_nc()
    in_maps = []
    for b in range(B):
        m = {"x": x[b]}
        m.update(w)
        in_maps.append(m)
    res = run_bass_kernel_spmd(nc, in_maps, core_ids=list(range(B)))
    return np.stack([np.asarray(r["out"]) for r in res.results], axis=0).astype(np.float32)
```

```python
import contextlib
import numpy as np
import concourse.bass as bass
import concourse.mybir as mybir
from concourse.bass_utils import run_bass_kernel_spmd

F32 = mybir.dt.float32
BF16 = mybir.dt.bfloat16
I32 = mybir.dt.int32
AF = mybir.ActivationFunctionType
ALU = mybir.AluOpType
GELU = AF.Gelu_apprx_tanh

S = 4096
D = 1024
NT = S // 128
EPS = 1e-6
ENGS = ("sync", "scalar", "vector", "gpsimd", "tensor")


class Buf:
    __slots__ = ("name", "writers", "readers", "unord", "pre")

    def __init__(self, name="b"):
        self.name = name
        self.writers = []
        self.readers = []
        self.unord = False
        self.pre = []


class Op:
    __slots__ = ("eng", "fn", "deps", "is_dma", "signal", "sigval", "sem", "idx")

    def __init__(self, eng, fn, is_dma):
        self.eng = eng
        self.fn = fn
        self.deps = []
        self.is_dma = is_dma
        self.signal = False
        self.sigval = None
        self.sem = None
        self.idx = None


class Prog:
    def __init__(self, nc, stack, n_dma_sems=64):
        self.nc = nc
        self.n_dma_sems = n_dma_sems
        self.esem = {e: stack.enter_context(nc.semaphore("es_" + e)) for e in ENGS}
        self.dsem = [stack.enter_context(nc.semaphore("ds_%d" % i)) for i in range(n_dma_sems)]
        self.ecount = {e: 0 for e in ENGS}
        self.dcount = [0] * n_dma_sems
        self.seen = {e: {} for e in ENGS}
        self.bufs = []
        self.nops = 0
        self._reset_phase()

    def _reset_phase(self):
        self.ops = {e: [] for e in ENGS}
        self.dma_rr = 0
        self.dma_last = [None] * self.n_dma_sems
        self.last_compute = {e: None for e in ENGS}
        for b in self.bufs:
            b.writers = []
            b.readers = []
            b.unord = False
            b.pre = []

    def buf(self, name="b"):
        b = Buf(name)
        self.bufs.append(b)
        return b

    def _add(self, eng, fn, reads, writes, uwrites, is_dma):
        op = Op(eng, fn, is_dma)
        op.idx = self.nops
        self.nops += 1
        deps = []
        for b in reads:
            deps.extend(b.writers)
        for b in writes:
            deps.extend(b.writers)
            deps.extend(b.readers)
        for b in uwrites:
            if not (b.unord and not b.readers):
                b.pre = list(b.writers) + list(b.readers)
                b.writers = []
                b.readers = []
                b.unord = True
            deps.extend(b.pre)
        if is_dma:
            s = self.dma_rr
            self.dma_rr = (self.dma_rr + 1) % self.n_dma_sems
            op.sem = s
            prev = self.dma_last[s]
            if prev is not None:
                deps.append(prev)
            self.dma_last[s] = op
        seen = set()
        for d in deps:
            if d is op or id(d) in seen:
                continue
            seen.add(id(d))
            if (not d.is_dma) and d.eng == "tensor" and eng == "tensor" and not is_dma:
                continue
            op.deps.append(d)
            if not d.is_dma:
                d.signal = True
        for b in reads:
            b.readers.append(op)
        for b in writes:
            b.writers = [op]
            b.readers = []
            b.unord = False
        for b in uwrites:
            b.writers.append(op)
        self.ops[eng].append(op)
        if not is_dma:
            self.last_compute[eng] = op
        return op

    def op(self, eng, fn, reads=(), writes=(), uwrites=()):
        return self._add(eng, fn, reads, writes, uwrites, False)

    def dma(self, eng, fn, reads=(), writes=(), uwrites=()):
        return self._add(eng, fn, reads, writes, uwrites, True)

    def end_phase(self):
        deps = [o for o in self.last_compute.values() if o is not None]
        deps += [o for o in self.dma_last if o is not None]
        for d in deps:
            if not d.is_dma:
                d.signal = True
        for e in ENGS:
            op = Op(e, None, False)
            op.idx = self.nops
            self.nops += 1
            op.deps = list(deps)
            self.ops[e].append(op)
        for e in ENGS:
            c = self.ecount[e]
            for o in self.ops[e]:
                if (not o.is_dma) and o.signal:
                    c += 1
                    o.sigval = c
            self.ecount[e] = c
        allops = []
        for e in ENGS:
            allops.extend(self.ops[e])
        allops.sort(key=lambda o: o.idx)
        for o in allops:
            if o.is_dma:
                self.dcount[o.sem] += 16
                o.sigval = self.dcount[o.sem]

        def run(e, h):
            seen = self.seen[e]
            for o in self.ops[e]:
                for d in o.deps:
                    if d.is_dma:
                        key = ("d", d.sem)
                        s = self.dsem[d.sem]
                    else:
                        if d.eng == e and d.fn is None:
                            continue
                        key = ("e", d.eng)
                        s = self.esem[d.eng]
                    if seen.get(key, 0) >= d.sigval:
                        continue
                    seen[key] = d.sigval
                    h.wait_ge(s, d.sigval)
                if o.fn is None:
                    continue
                ins = o.fn(h)
                if o.is_dma:
                    ins.then_inc(self.dsem[o.sem], 16)
                elif o.signal:
                    ins.then_inc(self.esem[e], 1)

        with self.nc.Block() as block:
            @block.sync
            def _(h):
                run("sync", h)

            @block.scalar
            def _(h):
                run("scalar", h)

            @block.vector
            def _(h):
                run("vector", h)

            @block.gpsimd
            def _(h):
                run("gpsimd", h)

            @block.tensor
            def _(h):
                run("tensor", h)
        self._reset_phase()


class Rot:
    def __init__(self, P, tiles):
        self.tiles = tiles
        self.bufs = [P.buf() for _ in tiles]
        self.i = 0

    def next(self):
        t, b = self.tiles[self.i], self.bufs[self.i]
        self.i = (self.i + 1) % len(self.tiles)
        return t, b


class K:
    def __init__(self, nc, stack, layers=(0, 1, 2, 3), sub=("mix", "moe")):
        self.nc = nc
        self.pid = 0
        self.P = Prog(nc, stack)
        self.layers = layers
        self.sub = sub
        dt = nc.dram_tensor
        self.x_in = dt("x", [S, D], F32, kind="ExternalInput")
        self.mix_norm = dt("mix_norm", [4, D], F32, kind="ExternalInput")
        self.ffn_norm = dt("ffn_norm", [4, D], F32, kind="ExternalInput")
        self.gm_w_in = dt("gm_w_in", [2, D, 6144], F32, kind="ExternalInput")
        self.gm_b_in = dt("gm_b_in", [2, 6144], F32, kind="ExternalInput")
        self.gm_v_norm = dt("gm_v_norm", [2, 3072], F32, kind="ExternalInput")
        self.gm_w_s = dt("gm_w_s", [2, 8, 128, 128], F32, kind="ExternalInput")
        self.gm_b_s = dt("gm_b_s", [2, 8, 128], F32, kind="ExternalInput")
        self.gm_w_out = dt("gm_w_out", [2, 3072, D], F32, kind="ExternalInput")
        self.gm_b_out = dt("gm_b_out", [2, D], F32, kind="ExternalInput")
        self.at_w_qkv = dt("at_w_qkv", [2, D, 4608], F32, kind="ExternalInput")
        self.at_q_norm = dt("at_q_norm", [2, 3, 64], F32, kind="ExternalInput")
        self.at_k_norm = dt("at_k_norm", [2, 3, 64], F32, kind="ExternalInput")
        self.at_w_o = dt("at_w_o", [2, 512, D], F32, kind="ExternalInput")
        self.moe_w_router = dt("moe_w_router", [4, D, 16], F32, kind="ExternalInput")
        self.moe_b_router = dt("moe_b_router", [4, 16], F32, kind="ExternalInput")
        self.moe_w1 = dt("moe_w1", [4, 16, D, 2048], F32, kind="ExternalInput")
        self.moe_w3 = dt("moe_w3", [4, 16, D, 2048], F32, kind="ExternalInput")
        self.moe_w2 = dt("moe_w2", [4, 16, 2048, D], F32, kind="ExternalInput")
        self.out = dt("out", [S, D], F32, kind="ExternalOutput")
        self.hT_D = dt("hT_D", [NT, 128, 8, 128], BF16)
        self.sT_D = dt("sT_D", [NT, 128, 24, 128], BF16)
        self.N_D = [dt("N_D%d" % g, [S, 520], F32) for g in range(3)]
        self.hD = dt("hD", [S, 1056], BF16)
        self.cD = dt("cD", [16, S], F32)
        self.idxD = dt("idxD", [128, 64], I32)
        sb = lambda n, s, d: stack.enter_context(nc.sbuf_tensor("p%d_%s" % (self.pid, n), s, d))
        self.ident_bf = sb("ident_bf", [128, 128], BF16)
        self.ident_f = sb("ident_f", [128, 128], F32)
        self.blockones = sb("blockones", [128, 128], BF16)
        self.b_const = self.P.buf("const")
        self.pid = 0

    def consts(self):
        P = self.P
        ib, if_, bo, bc = self.ident_bf, self.ident_f, self.blockones, self.b_const
        P.op("gpsimd", lambda h: h.memset(if_[:], 1.0), writes=[bc])
        P.op("gpsimd", lambda h: h.affine_select(out=if_[:], in_=if_[:], pattern=[[-1, 128]],
                                                  compare_op=ALU.is_equal, fill=0.0, base=0,
                                                  channel_multiplier=1), reads=[bc], writes=[bc])
        P.op("vector", lambda h: h.tensor_copy(out=ib[:], in_=if_[:]), reads=[bc], writes=[bc])
        P.op("vector", lambda h: h.memset(bo[:], 0.0), reads=[bc], writes=[bc])
        P.op("vector", lambda h: h.memset(bo[0:64, 0:64], 1.0), reads=[bc], writes=[bc])
        P.op("vector", lambda h: h.memset(bo[64:128, 64:128], 1.0), reads=[bc], writes=[bc])

    def cast_load(self, dst, src, wbuf, max_bytes=4 << 20):
        P = self.P
        _, kk, n = dst.shape
        ncol = n
        while ncol > 2048:
            ncol //= 2
        kstep = max(1, min(kk, max_bytes // (128 * ncol * 4)))
        for k0 in range(0, kk, kstep):
            k1 = min(kk, k0 + kstep)
            for c0 in range(0, n, ncol):
                P.dma("gpsimd", lambda h, a=dst[:, k0:k1, c0:c0 + ncol], b=src[:, k0:k1, c0:c0 + ncol]:
                      h.dma_start(out=a, in_=b), uwrites=[wbuf])

    def bcast_load(self, dst, src_row, wbuf, eng="sync"):
        self.P.dma(eng, lambda h: h.dma_start(out=dst, in_=src_row.partition_broadcast(128)), writes=[wbuf])

    def rows_ap(self, t, ncols, start, step, nrows=128):
        return bass.AP(t, start * ncols, [[step * ncols, nrows], [1, ncols]])

    def norm_tile(self, xt, bx, gain_bc, bgain, W, hb_dtype=BF16):
        P = self.P
        junk, bjunk = W["junk"].next()
        st, bst = W["stat"].next()
        hb, bhb = W["hb"].next()
        P.op("scalar", lambda h: h.activation(out=junk[:, 0:D], in_=xt, func=AF.Square, accum_out=st[:, 0:1]),
             reads=[bx], writes=[bjunk, bst])
        P.op("scalar", lambda h: h.activation(out=st[:, 1:2], in_=st[:, 0:1], func=AF.Ln, scale=1.0 / D, bias=self.eps_t[:, 0:1]),
             reads=[bst, self.b_const], writes=[bst])
        P.op("scalar", lambda h: h.activation(out=st[:, 2:3], in_=st[:, 1:2], func=AF.Exp, scale=-0.5), reads=[bst], writes=[bst])
        P.op("vector", lambda h: h.scalar_tensor_tensor(out=hb[:], in0=xt, scalar=st[:, 2:3], in1=gain_bc[:],
                                                        op0=ALU.mult, op1=ALU.mult),
             reads=[bx, bst, bgain], writes=[bhb])
        return hb, bhb

    def transpose8(self, hb, bhb, dst, bdst, W, eng="vector"):
        P = self.P
        pT, bpT = W["pT"].next()
        for k in range(8):
            P.op("tensor", lambda h, k=k: h.transpose(out=pT[:, k, :], in_=hb[:, k * 128:(k + 1) * 128],
                                                      identity=self.ident_bf[:]),
                 reads=[bhb, self.b_const], writes=[bpT])
        if eng == "vector":
            P.op("vector", lambda h: h.tensor_copy(out=dst, in_=pT[:]), reads=[bpT], writes=[bdst])
        else:
            P.op("scalar", lambda h: h.copy(out=dst, in_=pT[:]), reads=[bpT], writes=[bdst])

    def phase(self):
        self.pid += 1
        return contextlib.ExitStack()

    def gmlp(self, i, src):
        nc, P = self.nc, self.P
        j = i // 2
        with self.phase() as st:
            sb = lambda n, s, d: st.enter_context(nc.sbuf_tensor("p%d_%s" % (self.pid, n), s, d))
            ps = lambda n, s, d: st.enter_context(nc.psum_tensor("p%d_%s" % (self.pid, n), s, d))
            wv = sb("wv", [128, 8, 3072], BF16); bwv = [P.buf() for _ in range(6)]
            gain_bc = sb("gain_bc", [128, D], F32); bgain = P.buf()
            bv_bc = sb("bv_bc", [128, 3072], F32); bbv = P.buf()
            wsf = sb("wsf", [128, 8, 128], F32); wsb = sb("wsb", [128, 8, 128], BF16)
            wsT = sb("wsT", [128, 8, 128], BF16); bws = P.buf()
            vgT = sb("vgT", [128, 24], F32); bvg = P.buf()
            bs_bc = sb("bs_bc", [128, 8, 128], F32); bbs = P.buf()
            self.eps_t = sb("eps_t", [128, 1], F32)
            P.op("vector", lambda h: h.memset(self.eps_t[:], EPS), reads=[self.b_const], writes=[self.b_const])
            W = {
                "junk": Rot(P, [sb("junk", [128, 3072], BF16)]),
                "stat": Rot(P, [sb("stat%d" % q, [128, 4], F32) for q in range(4)]),
                "hb": Rot(P, [sb("hb%d" % q, [128, D], BF16) for q in range(3)]),
                "pT": Rot(P, [ps("pT%d" % q, [128, 8, 128], BF16) for q in range(2)]),
            }
            xts = Rot(P, [sb("xt%d" % q, [128, D], F32) for q in range(4)])
            hTs = Rot(P, [sb("hT%d" % q, [128, 8, 128], BF16) for q in range(3)])
            pvs = Rot(P, [ps("pv%d" % q, [128, 512], F32) for q in range(3)])
            tmps = Rot(P, [sb("tmp%d" % q, [128, 512], F32) for q in range(4)])
            vs = Rot(P, [sb("v%d" % q, [128, 3072], BF16) for q in range(3)])
            vns = Rot(P, [sb("vn%d" % q, [128, 3072], BF16) for q in range(3)])
            pss = Rot(P, [ps("pss%d" % q, [128, 4, 128], F32) for q in range(3)])
            sTs = Rot(P, [sb("sT%d" % q, [128, 24, 128], BF16) for q in range(3)])
            stv = Rot(P, [sb("stv%d" % q, [128, 4], F32) for q in range(4)])
            self.bcast_load(gain_bc[:], self.mix_norm[i], bgain)
            self.bcast_load(bv_bc[:], self.gm_b_in[j, 3072:6144], bbv)
            self.bcast_load(bs_bc[:].rearrange("p g t -> p (g t)"), self.gm_b_s[j].rearrange("g t -> (g t)"), bbs)
            P.dma("sync", lambda h: h.dma_start(out=vgT[:], in_=self.gm_v_norm[j].rearrange("(c p) -> p c", p=128),
                                                allow_slow_non_contiguous=True), writes=[bvg])
            P.dma("sync", lambda h: h.dma_start(out=wsf[:], in_=self.gm_w_s[j].rearrange("g t s -> t g s")), writes=[bws])
            wvsrc = self.gm_w_in[j, :, 3072:6144].rearrange("(k p) f -> p k f", p=128)
            for blk in range(6):
                self.cast_load(wv[:, :, blk * 512:(blk + 1) * 512], wvsrc[:, :, blk * 512:(blk + 1) * 512], bwv[blk])
            P.op("vector", lambda h: h.tensor_copy(out=wsb[:], in_=wsf[:]), reads=[bws], writes=[bws])
            pT0, bpT0 = W["pT"].next()
            for g in range(8):
                P.op("tensor", lambda h, g=g: h.transpose(out=pT0[:, g, :], in_=wsb[:, g, :], identity=self.ident_bf[:]),
                     reads=[bws, self.b_const], writes=[bpT0])
            P.op("vector", lambda h: h.tensor_copy(out=wsT[:], in_=pT0[:]), reads=[bpT0, bws], writes=[bws])
            stV = {}

            stL = {}

            def stage_l(t):
                xt, bx = xts.next()
                P.dma("sync", lambda h, xt=xt, t=t: h.dma_start(out=xt[:], in_=src[t * 128:(t + 1) * 128, :]), writes=[bx])
                stL[t] = (xt, bx)

            def stage_n(t):
                xt, bx = stL.pop(t)
                hb, bhb = self.norm_tile(xt[:], bx, gain_bc, bgain, W)
                hT, bhT = hTs.next()
                self.transpose8(hb, bhb, hT[:], bhT, W)
                P.dma("sync", lambda h, hT=hT, t=t: h.dma_start(out=self.hT_D[t], in_=hT[:]), reads=[bhT])
                stV[t] = [hT, bhT]

            def stage_m(t):
                hT, bhT = stV[t]
                v, bv = vs.next()
                for blk in range(6):
                    pv, bpv = pvs.next()
                    for k in range(8):
                        P.op("tensor", lambda h, pv=pv, hT=hT, k=k, blk=blk: h.matmul(
                            pv[:], lhsT=hT[:, k, :], rhs=wv[:, k, blk * 512:(blk + 1) * 512], start=(k == 0), stop=(k == 7)),
                            reads=[bhT, bwv[blk]], writes=[bpv])
                    tmp, btmp = tmps.next()
                    P.op("vector", lambda h, tmp=tmp, pv=pv, blk=blk: h.tensor_tensor(
                        out=tmp[:], in0=pv[:], in1=bv_bc[:, blk * 512:(blk + 1) * 512], op=ALU.add),
                        reads=[bpv, bbv], writes=[btmp])
                    P.op("scalar", lambda h, tmp=tmp, v=v, blk=blk: h.activation(
                        out=v[:, blk * 512:(blk + 1) * 512], in_=tmp[:], func=GELU), reads=[btmp], writes=[bv])
                stV[t] = [v, bv]

            def stage_b(t):
                v, bv = stV.pop(t)
                junk, bjunk = W["junk"].next()
                sv, bsv = stv.next()
                P.op("scalar", lambda h, junk=junk, v=v, sv=sv: h.activation(out=junk[:], in_=v[:], func=AF.Square, accum_out=sv[:, 0:1]),
                     reads=[bv], writes=[bjunk, bsv])
                P.op("scalar", lambda h, sv=sv: h.activation(out=sv[:, 1:2], in_=sv[:, 0:1], func=AF.Ln, scale=1.0 / 3072, bias=self.eps_t[:, 0:1]),
                     reads=[bsv, self.b_const], writes=[bsv])
                P.op("scalar", lambda h, sv=sv: h.activation(out=sv[:, 2:3], in_=sv[:, 1:2], func=AF.Exp, scale=-0.5), reads=[bsv], writes=[bsv])
                vn, bvn = vns.next()
                P.op("vector", lambda h, vn=vn, v=v, sv=sv: h.tensor_scalar(out=vn[:], in0=v[:], scalar1=sv[:, 2:3], scalar2=None, op0=ALU.mult),
                     reads=[bv, bsv], writes=[bvn])
                sT, bsT = sTs.next()
                for q in range(6):
                    pq, bpq = pss.next()
                    for m in range(4):
                        fc = q * 4 + m
                        P.op("tensor", lambda h, pq=pq, vn=vn, fc=fc, m=m: h.matmul(
                            pq[:, m, :], lhsT=vn[:, fc * 128:(fc + 1) * 128], rhs=wsT[:, fc // 3, :], start=True, stop=True),
                            reads=[bvn, bws], writes=[bpq])
                    for m in range(4):
                        fc = q * 4 + m
                        P.op("vector", lambda h, pq=pq, sT=sT, fc=fc, m=m: h.scalar_tensor_tensor(
                            out=sT[:, fc, :], in0=pq[:, m, :], scalar=vgT[:, fc:fc + 1], in1=bs_bc[:, fc // 3, :],
                            op0=ALU.mult, op1=ALU.add), reads=[bpq, bvg, bbs], writes=[bsT])
                P.dma("sync", lambda h, sT=sT, t=t: h.dma_start(out=self.sT_D[t], in_=sT[:]), reads=[bsT])

            stage_l(0)
            stage_l(1)
            stage_l(2)
            stage_n(0)
            stage_n(1)
            stage_m(0)
            for t in range(NT):
                if t + 3 < NT:
                    stage_l(t + 3)
                if t + 2 < NT:
                    stage_n(t + 2)
                if t + 1 < NT:
                    stage_m(t + 1)
                stage_b(t)
            P.end_phase()
        with self.phase() as st:
            sb = lambda n, s, d: st.enter_context(nc.sbuf_tensor("p%d_%s" % (self.pid, n), s, d))
            ps = lambda n, s, d: st.enter_context(nc.psum_tensor("p%d_%s" % (self.pid, n), s, d))
            wu = sb("wu", [128, 8, 3072], BF16); bwu = [P.buf() for _ in range(6)]
            wo = sb("wo", [128, 24, D], BF16); bwo = [P.buf() for _ in range(4)]
            buT = sb("buT", [128, 24], F32); bbu = P.buf()
            bout_bc = sb("bout_bc", [128, D], F32); bbo = P.buf()
            hTss = Rot(P, [sb("hTs%d" % q, [128, 2, 8, 128], BF16) for q in range(2)])
            sTss = Rot(P, [sb("sTs%d" % q, [128, 2, 24, 128], BF16) for q in range(2)])
            xss = Rot(P, [sb("xs%d" % q, [128, 2, D], F32) for q in range(2)])
            xns = Rot(P, [sb("xn%d" % q, [128, 2, D], F32) for q in range(2)])
            gTs = Rot(P, [sb("gT", [128, 24, 2, 128], BF16)])
            uts = Rot(P, [sb("ut%d" % q, [128, 2, 128], BF16) for q in range(2)])
            pus = Rot(P, [ps("pu%d" % q, [128, 2, 128], F32) for q in range(3)])
            pos = Rot(P, [ps("po%d" % q, [128, 512], F32) for q in range(2)])
            self.bcast_load(bout_bc[:], self.gm_b_out[j], bbo)
            P.dma("sync", lambda h: h.dma_start(out=buT[:], in_=self.gm_b_in[j, 0:3072].rearrange("(c p) -> p c", p=128),
                                                allow_slow_non_contiguous=True), writes=[bbu])
            wusrc = self.gm_w_in[j, :, 0:3072].rearrange("(k p) f -> p k f", p=128)
            wosrc = self.gm_w_out[j].rearrange("(c p) o -> p c o", p=128)
            for blk in range(6):
                self.cast_load(wu[:, :, blk * 512:(blk + 1) * 512], wusrc[:, :, blk * 512:(blk + 1) * 512], bwu[blk])
            for blk in range(4):
                self.cast_load(wo[:, blk * 6:(blk + 1) * 6, :], wosrc[:, blk * 6:(blk + 1) * 6, :], bwo[blk])
            stU = {}

            def load_u(s):
                hTs_, bh = hTss.next()
                sTs_, bs_ = sTss.next()
                xs, bxs = xss.next()
                P.dma("sync", lambda h, a=hTs_, s=s: h.dma_start(out=a[:], in_=self.hT_D[2 * s:2 * s + 2].rearrange("t p k n -> p t k n")), writes=[bh])
                P.dma("sync", lambda h, a=sTs_, s=s: h.dma_start(out=a[:], in_=self.sT_D[2 * s:2 * s + 2].rearrange("t p c n -> p t c n")), writes=[bs_])
                P.dma("sync", lambda h, a=xs, s=s: h.dma_start(out=a[:], in_=src[s * 256:(s + 1) * 256, :].rearrange("(t p) d -> p t d", p=128)), writes=[bxs])
                stU[s] = (hTs_, bh, sTs_, bs_, xs, bxs)

            load_u(0)
            for s in range(NT // 2):
                if s + 1 < NT // 2:
                    load_u(s + 1)
                hTs_, bh, sTs_, bs_, xs, bxs = stU.pop(s)
                for t in range(2):
                    P.op("gpsimd", lambda h, xs=xs, t=t: h.tensor_tensor(out=xs[:, t, :], in0=xs[:, t, :], in1=bout_bc[:], op=ALU.add),
                         reads=[bxs, bbo], writes=[bxs])
                gT, bgT = gTs.next()
                for fc in range(24):
                    pu, bpu = pus.next()
                    for k in range(8):
                        P.op("tensor", lambda h, pu=pu, a=hTs_, k=k, fc=fc: h.matmul(
                            pu[:], lhsT=wu[:, k, fc * 128:(fc + 1) * 128], rhs=a[:, :, k, :], start=(k == 0), stop=(k == 7)),
                            reads=[bwu[fc // 4], bh], writes=[bpu])
                    ut, but = uts.next()
                    P.op("scalar", lambda h, pu=pu, ut=ut, fc=fc: h.activation(out=ut[:], in_=pu[:], func=GELU, bias=buT[:, fc:fc + 1]),
                         reads=[bpu, bbu], writes=[but])
                    P.op("vector", lambda h, ut=ut, gT=gT, a=sTs_, fc=fc: h.tensor_tensor(
                        out=gT[:, fc, :, :], in0=ut[:], in1=a[:, :, fc, :], op=ALU.mult), reads=[but, bs_], writes=[bgT])
                xn, bxn = xns.next()
                for t in range(2):
                    for ob in range(2):
                        po, bpo = pos.next()
                        for fc in range(24):
                            P.op("tensor", lambda h, po=po, gT=gT, fc=fc, t=t, ob=ob: h.matmul(
                                po[:], lhsT=gT[:, fc, t, :], rhs=wo[:, fc, ob * 512:(ob + 1) * 512], start=(fc == 0), stop=(fc == 23)),
                                reads=[bgT, bwo[fc // 6]], writes=[bpo])
                        P.op("vector", lambda h, po=po, xn=xn, xs=xs, t=t, ob=ob: h.tensor_tensor(
                            out=xn[:, t, ob * 512:(ob + 1) * 512], in0=po[:], in1=xs[:, t, ob * 512:(ob + 1) * 512], op=ALU.add),
                            reads=[bpo, bxs], writes=[bxn])
                P.dma("sync", lambda h, xn=xn, s=s: h.dma_start(out=self.out[s * 256:(s + 1) * 256, :].rearrange("(t p) d -> p t d", p=128), in_=xn[:]),
                      reads=[bxn])
            P.end_phase()

    def attn(self, i, src):
        nc, P = self.nc, self.P
        j = i // 2
        dils = (1, 4, 16)
        with self.phase() as st:
            sb = lambda n, s, d: st.enter_context(nc.sbuf_tensor("p%d_%s" % (self.pid, n), s, d))
            ps = lambda n, s, d: st.enter_context(nc.psum_tensor("p%d_%s" % (self.pid, n), s, d))
            gain_bc = sb("gain_bc", [128, D], F32); bgain = P.buf()
            self.eps_t = sb("eps_t", [128, 1], F32)
            P.op("vector", lambda h: h.memset(self.eps_t[:], EPS), reads=[self.b_const], writes=[self.b_const])
            wq = sb("wq", [128, 8, 512], BF16); wk = sb("wk", [128, 8, 512], BF16); wvv = sb("wvv", [128, 8, 512], BF16)
            bwq, bwk, bwvv = P.buf(), P.buf(), P.buf()
            gq = sb("gq", [128, 1], F32); gk = sb("gk", [128, 1], F32); bg = P.buf()
            QT = sb("QT", [128, 4, S], BF16); KT = sb("KT", [128, 4, S], BF16)
            V = sb("V", [128, NT, 8, 65], BF16)
            bQT, bKT, bV = P.buf(), P.buf(), P.buf()
            E = sb("E", [128, 3, 8, 128], BF16); bE = P.buf()
            rel = sb("rel", [128, 3, 128], F32); reli = sb("reli", [128, 3, 128], I32)
            msk = sb("msk", [128, 3, 128], F32); etmp = sb("etmp", [128, 3, 128], F32); brel = P.buf()
            W = {
                "junk": Rot(P, [sb("junk", [128, D], BF16)]),
                "stat": Rot(P, [sb("stat%d" % q, [128, 4], F32) for q in range(4)]),
                "hb": Rot(P, [sb("hb%d" % q, [128, D], BF16) for q in range(3)]),
                "pT": Rot(P, [ps("pT%d" % q, [128, 8, 128], BF16) for q in range(1)]),
            }
            xts = Rot(P, [sb("xt%d" % q, [128, D], F32) for q in range(3)])
            hTbs = Rot(P, [sb("hTb%d" % q, [128, 8, 512], BF16) for q in range(2)])
            pqs = Rot(P, [ps("pq%d" % q, [128, 512], F32) for q in range(4)])
            pns = Rot(P, [ps("pn%d" % q, [128, 512], F32) for q in range(1)])
            sqs = Rot(P, [sb("sq%d" % q, [128, 512], BF16) for q in range(3)])
            stds = Rot(P, [sb("std%d" % q, [128, 512], F32) for q in range(3)])
            pSs = pqs
            pOs = [ps("pO%d" % q, [128, 4, 65], F32) for q in range(2)]
            bpOs = [P.buf(), P.buf()]
            exs = Rot(P, [sb("ex%d" % q, [128, 4, 128], BF16) for q in range(3)])
            pTs = Rot(P, [sb("pTt%d" % q, [128, 4, 128], BF16) for q in range(8)])
            Obs = Rot(P, [sb("Ob%d" % q, [128, 8, 65], F32) for q in range(2)])
            self.bcast_load(gain_bc[:], self.mix_norm[i], bgain)
            P.op("gpsimd", lambda h: h.iota(reli[:], pattern=[[128, 3], [-1, 128]], base=-128, channel_multiplier=1), writes=[brel])
            P.op("vector", lambda h: h.tensor_copy(out=rel[:], in_=reli[:]), reads=[brel], writes=[brel])
            P.op("scalar", lambda h: h.activation(out=rel[:], in_=rel[:], func=AF.Abs), reads=[brel], writes=[brel])
            P.op("vector", lambda h: h.tensor_single_scalar(out=msk[:], in_=rel[:], scalar=64.5, op=ALU.is_le), reads=[brel], writes=[brel])
            P.op("gpsimd", lambda h: h.memset(V[:, :, :, 64:65], 1.0), writes=[bV])
            for g in range(3):
                d = dils[g]
                L = S // d
                nb = L // 128
                wsrc = self.at_w_qkv[j].rearrange("(k p) f -> p k f", p=128)
                self.cast_load(wq[:], wsrc[:, :, g * 512:(g + 1) * 512], bwq)
                self.cast_load(wk[:], wsrc[:, :, 1536 + g * 512:1536 + (g + 1) * 512], bwk)
                self.cast_load(wvv[:], wsrc[:, :, 3072 + g * 512:3072 + (g + 1) * 512], bwvv)
                for half in range(2):
                    P.dma("sync", lambda h, half=half, g=g: h.dma_start(out=gq[half * 64:(half + 1) * 64, :], in_=self.at_q_norm[j, g].rearrange("(p o) -> p o", o=1)), uwrites=[bg])
                    P.dma("sync", lambda h, half=half, g=g: h.dma_start(out=gk[half * 64:(half + 1) * 64, :], in_=self.at_k_norm[j, g].rearrange("(p o) -> p o", o=1)), uwrites=[bg])
                P.op("vector", lambda h: h.tensor_scalar(out=gq[:], in0=gq[:], scalar1=0.125, scalar2=None, op0=ALU.mult), reads=[bg], writes=[bg])
                for ei in range(8):
                    hd = 2 * (ei % 4) + ei // 4
                    slope = 2.0 ** (-(hd + 1))
                    P.op("scalar", lambda h, slope=slope, d=d: h.activation(out=etmp[:], in_=rel[:], func=AF.Exp, scale=-slope * d), reads=[brel], writes=[brel])
                    P.op("vector", lambda h, ei=ei: h.tensor_tensor(out=E[:, :, ei, :], in0=etmp[:], in1=msk[:], op=ALU.mult), reads=[brel], writes=[bE])
                stP = {}

                def stage_pn(pb, d=d, nb=nb):
                    hTb, bhTb = hTbs.next()
                    for q in range(4):
                        pt = pb * 4 + q
                        r, b = pt // nb, pt % nb
                        xt, bx = xts.next()
                        P.dma("sync", lambda h, xt=xt, r=r, b=b, d=d: h.dma_start(out=xt[:], in_=self.rows_ap(src, D, r + b * 128 * d, d)), writes=[bx])
                        hb, bhb = self.norm_tile(xt[:], bx, gain_bc, bgain, W)
                        self.transpose8(hb, bhb, hTb[:, :, q * 128:(q + 1) * 128], bhTb, W)
                    stP[pb] = (hTb, bhTb)

                def qk_finish(pend):
                    pq, bpq, sq, bsq, gt, dstT, bdst, c, pb = pend
                    pn, bpn = pns.next()
                    P.op("tensor", lambda h, pn=pn, sq=sq: h.matmul(pn[:], lhsT=self.blockones[:], rhs=sq[:], start=True, stop=True),
                         reads=[bsq, self.b_const], writes=[bpn])
                    sd, bsd = stds.next()
                    P.op("scalar", lambda h, sd=sd, pn=pn: h.activation(out=sd[:], in_=pn[:], func=AF.Ln, scale=1.0 / 64, bias=self.eps_t[:, 0:1]),
                         reads=[bpn, self.b_const], writes=[bsd])
                    P.op("scalar", lambda h, sd=sd: h.activation(out=sd[:], in_=sd[:], func=AF.Exp, scale=-0.5), reads=[bsd], writes=[bsd])
                    P.op("vector", lambda h, sd=sd, pq=pq, gt=gt, dstT=dstT, c=c, pb=pb: h.scalar_tensor_tensor(
                        out=dstT[:, c, pb * 512:(pb + 1) * 512], in0=pq[:], scalar=gt[:, 0:1], in1=sd[:], op0=ALU.mult, op1=ALU.mult),
                        reads=[bpq, bsd, bg], uwrites=[bdst])

                def stage_pm(pb):
                    hTb, bhTb = stP.pop(pb)
                    pend = None
                    for (wt, bwt, gt, dstT, bdst) in ((wq, bwq, gq, QT, bQT), (wk, bwk, gk, KT, bKT)):
                        for c in range(4):
                            pq, bpq = pqs.next()
                            for k in range(8):
                                P.op("tensor", lambda h, pq=pq, wt=wt, hTb=hTb, k=k, c=c: h.matmul(
                                    pq[:], lhsT=wt[:, k, c * 128:(c + 1) * 128], rhs=hTb[:, k, :], start=(k == 0), stop=(k == 7)),
                                    reads=[bwt, bhTb], writes=[bpq])
                            sq, bsq = sqs.next()
                            P.op("scalar", lambda h, sq=sq, pq=pq: h.activation(out=sq[:], in_=pq[:], func=AF.Square), reads=[bpq], writes=[bsq])
                            if pend is not None:
                                qk_finish(pend)
                            pend = (pq, bpq, sq, bsq, gt, dstT, bdst, c, pb)
                    for q in range(4):
                        pt = pb * 4 + q
                        pq, bpq = pqs.next()
                        for k in range(8):
                            P.op("tensor", lambda h, pq=pq, hTb=hTb, k=k, q=q: h.matmul(
                                pq[:], lhsT=hTb[:, k, q * 128:(q + 1) * 128], rhs=wvv[:, k, :], start=(k == 0), stop=(k == 7)),
                                reads=[bwvv, bhTb], writes=[bpq])
                        if pend is not None:
                            qk_finish(pend)
                            pend = None
                        P.op("vector", lambda h, pq=pq, pt=pt: h.tensor_copy(out=V[:, pt, :, 0:64], in_=pq[:].rearrange("p (a b) -> p a b", b=64)),
                             reads=[bpq], uwrites=[bV])

                stage_pn(0)
                for pb in range(8):
                    if pb + 1 < 8:
                        stage_pn(pb + 1)
                    stage_pm(pb)
                for qb in range(NT):
                    r, b = qb // nb, qb % nb
                    offs = [o for o in (-1, 0, 1) if 0 <= b + o < nb]
                    pTl = {}
                    for hh in range(2):
                        for o in offs:
                            kt = qb + o
                            pS, bpS = pSs.next()
                            for m in range(4):
                                hd = 2 * m + hh
                                c, hp = hd // 2, hd % 2
                                P.op("tensor", lambda h, pS=pS, m=m, c=c, hp=hp, kt=kt, qb=qb: h.matmul(
                                    pS[:, m * 128:(m + 1) * 128], lhsT=KT[hp * 64:(hp + 1) * 64, c, kt * 128:(kt + 1) * 128],
                                    rhs=QT[hp * 64:(hp + 1) * 64, c, qb * 128:(qb + 1) * 128], start=True, stop=True),
                                    reads=[bKT, bQT], writes=[bpS])
                            ex, bex = exs.next()
                            P.op("scalar", lambda h, ex=ex, pS=pS: h.activation(out=ex[:].rearrange("p a b -> p (a b)"), in_=pS[:], func=AF.Exp), reads=[bpS], writes=[bex])
                            pT_, bpT_ = pTs.next()
                            P.op("vector", lambda h, ex=ex, pT_=pT_, o=o, hh=hh: h.tensor_tensor(
                                out=pT_[:], in0=ex[:], in1=E[:, o + 1, hh * 4:(hh + 1) * 4, :], op=ALU.mult), reads=[bex, bE], writes=[bpT_])
                            pTl[(hh, o)] = (pT_, bpT_)
                    Ob, bOb = Obs.next()
                    for hh in range(2):
                        pO, bpO = pOs[hh], bpOs[hh]
                        for m in range(4):
                            hd = 2 * m + hh
                            for oi, o in enumerate(offs):
                                kt = qb + o
                                pT_, bpT_ = pTl[(hh, o)]
                                P.op("tensor", lambda h, pO=pO, pT_=pT_, m=m, kt=kt, hd=hd, oi=oi, n=len(offs): h.matmul(
                                    pO[:, m, :], lhsT=pT_[:, m, :], rhs=V[:, kt, hd, :], start=(oi == 0), stop=(oi == n - 1)),
                                    reads=[bpT_, bV], writes=[bpO])
                        P.op("vector" if hh == 0 else "scalar",
                             (lambda h, Ob=Ob, pO=pO, hh=hh: h.tensor_copy(out=Ob[:].rearrange("p (m b) c -> p m b c", b=2)[:, :, hh, :], in_=pO[:])) if hh == 0 else
                             (lambda h, Ob=Ob, pO=pO, hh=hh: h.copy(out=Ob[:].rearrange("p (m b) c -> p m b c", b=2)[:, :, hh, :], in_=pO[:])),
                             reads=[bpO], uwrites=[bOb])
                    P.dma("sync", lambda h, Ob=Ob, r=r, b=b, d=d, g=g: h.dma_start(
                        out=self.rows_ap(self.N_D[g], 520, r + b * 128 * d, d), in_=Ob[:].rearrange("p a b -> p (a b)")), reads=[bOb])
            P.end_phase()
        with self.phase() as st:
            sb = lambda n, s, d: st.enter_context(nc.sbuf_tensor("p%d_%s" % (self.pid, n), s, d))
            ps = lambda n, s, d: st.enter_context(nc.psum_tensor("p%d_%s" % (self.pid, n), s, d))
            wo = sb("wo", [128, 4, D], BF16); bwo = P.buf()
            self.cast_load(wo[:], self.at_w_o[j].rearrange("(c p) o -> p c o", p=128), bwo)
            Ns = [Rot(P, [sb("N%d_%d" % (g, q), [128, 8, 65], F32) for q in range(6)]) for g in range(3)]
            rds = Rot(P, [sb("rd%d" % q, [128, 8], F32) for q in range(4)])
            obs = Rot(P, [sb("ob%d" % q, [128, 8, 64], BF16) for q in range(4)])
            pT2 = Rot(P, [ps("pT2_%d" % q, [128, 4, 128], BF16) for q in range(2)])
            oTs = Rot(P, [sb("oT%d" % q, [128, 4, 128], BF16) for q in range(4)])
            xts = Rot(P, [sb("xt%d" % q, [128, D], F32) for q in range(7)])
            xns = Rot(P, [sb("xn%d" % q, [128, D], F32) for q in range(4)])
            pos = Rot(P, [ps("po%d" % q, [128, 512], F32) for q in range(4)])
            stG = {}

            stGL = {}

            def stage_gl(t):
                tl = []
                for g in range(3):
                    n_, bn = Ns[g].next()
                    P.dma("sync", lambda h, n_=n_, g=g, t=t: h.dma_start(out=n_[:].rearrange("p a b -> p (a b)"), in_=self.N_D[g][t * 128:(t + 1) * 128, :]), writes=[bn])
                    tl.append((n_, bn))
                xt, bx = xts.next()
                P.dma("sync", lambda h, xt=xt, t=t: h.dma_start(out=xt[:], in_=src[t * 128:(t + 1) * 128, :]), writes=[bx])
                stGL[t] = (tl, xt, bx)

            def stage_g0(t):
                tl, xt, bx = stGL.pop(t)
                n0, bn0 = tl[0]
                P.op("vector", lambda h, n0=n0, n1=tl[1][0]: h.tensor_tensor(out=n0[:], in0=n0[:], in1=n1[:], op=ALU.add), reads=[tl[1][1], bn0], writes=[bn0])
                P.op("vector", lambda h, n0=n0, n2=tl[2][0]: h.tensor_tensor(out=n0[:], in0=n0[:], in1=n2[:], op=ALU.add), reads=[tl[2][1], bn0], writes=[bn0])
                rd, brd = rds.next()
                P.op("vector", lambda h, rd=rd, n0=n0: h.reciprocal(out=rd[:], in_=n0[:, :, 64]), reads=[bn0], writes=[brd])
                ob, bob = obs.next()
                P.op("vector", lambda h, ob=ob, n0=n0, rd=rd: h.tensor_tensor(
                    out=ob[:], in0=n0[:, :, 0:64], in1=rd[:].unsqueeze(2).to_broadcast([128, 8, 64]), op=ALU.mult),
                    reads=[bn0, brd], writes=[bob])
                stG[t] = dict(xt=xt, bx=bx, ob=ob, bob=bob)

            def stage_g1(t):
                c_ = stG[t]
                ob, bob = c_["ob"], c_["bob"]
                pt_, bpt = pT2.next()
                for c in range(4):
                    P.op("tensor", lambda h, pt_=pt_, ob=ob, c=c: h.transpose(
                        out=pt_[:, c, :], in_=ob[:, 2 * c:2 * c + 2, :].rearrange("p a b -> p (a b)"), identity=self.ident_bf[:]),
                        reads=[bob, self.b_const], writes=[bpt])
                oT, boT = oTs.next()
                P.op("scalar", lambda h, oT=oT, pt_=pt_: h.copy(out=oT[:], in_=pt_[:]), reads=[bpt], writes=[boT])
                c_["oT"], c_["boT"] = oT, boT

            def stage_g2(t):
                c_ = stG.pop(t)
                xt, bx, oT, boT = c_["xt"], c_["bx"], c_["oT"], c_["boT"]
                xn, bxn = xns.next()
                for obk in range(2):
                    po, bpo = pos.next()
                    for c in range(4):
                        P.op("tensor", lambda h, po=po, oT=oT, c=c, obk=obk: h.matmul(
                            po[:], lhsT=oT[:, c, :], rhs=wo[:, c, obk * 512:(obk + 1) * 512], start=(c == 0), stop=(c == 3)),
                            reads=[boT, bwo], writes=[bpo])
                    P.op("vector", lambda h, po=po, xn=xn, xt=xt, obk=obk: h.tensor_tensor(
                        out=xn[:, obk * 512:(obk + 1) * 512], in0=po[:], in1=xt[:, obk * 512:(obk + 1) * 512], op=ALU.add),
                        reads=[bpo, bx], uwrites=[bxn])
                P.dma("sync", lambda h, xn=xn, t=t: h.dma_start(out=self.out[t * 128:(t + 1) * 128, :], in_=xn[:]), reads=[bxn])

            for step in range(NT + 4):
                if step < NT:
                    stage_gl(step)
                if 0 <= step - 2 < NT:
                    stage_g0(step - 2)
                if 0 <= step - 3 < NT:
                    stage_g1(step - 3)
                if 0 <= step - 4 < NT:
                    stage_g2(step - 4)
            P.end_phase()

    def moe(self, i):
        nc, P = self.nc, self.P
        src = self.out
        with self.phase() as st:
            sb = lambda n, s, d: st.enter_context(nc.sbuf_tensor("p%d_%s" % (self.pid, n), s, d))
            ps = lambda n, s, d: st.enter_context(nc.psum_tensor("p%d_%s" % (self.pid, n), s, d))
            gain_bc = sb("gain_bc", [128, D], F32); bgain = P.buf()
            self.eps_t = sb("eps_t", [128, 1], F32)
            P.op("vector", lambda h: h.memset(self.eps_t[:], EPS), reads=[self.b_const], writes=[self.b_const])
            wr = sb("wr", [128, 8, 16], F32); bwr = P.buf()
            br_bc = sb("br_bc", [128, 16], F32); bbr = P.buf()
            affT = sb("affT", [16, S], F32); baffT = P.buf()
            cjunk = sb("cjunk", [128, S], BF16); bcj = P.buf()
            ajunk = sb("ajunk", [128, S], BF16); baj = P.buf()
            lo = sb("lo", [16, 4], F32); blo = P.buf()
            jvi = sb("jvi", [128, 4], I32); jv = sb("jv", [128, 4], F32); jvh = sb("jvh", [128, 4], F32); bjv = P.buf()
            idxf = sb("idxf", [128, 64], F32); idxi = sb("idxi", [128, 64], I32); bidx = P.buf()
            xts = Rot(P, [sb("xt%d" % q, [128, D], F32) for q in range(5)])
            junks = Rot(P, [sb("junk%d" % q, [128, D], BF16) for q in range(2)])
            stats = Rot(P, [sb("stat%d" % q, [128, 8], F32) for q in range(6)])
            hfs = Rot(P, [sb("hf%d" % q, [128, D], F32) for q in range(4)])
            hbs = Rot(P, [sb("hbx%d" % q, [128, 1056], BF16) for q in range(6)])
            pTfs = Rot(P, [ps("pTf%d" % q, [128, 4, 128], F32) for q in range(4)])
            hT32s = Rot(P, [sb("hT32_%d" % q, [128, 8, 128], F32) for q in range(3)])
            prs = Rot(P, [ps("pr%d" % q, [128, 16], F32) for q in range(2)])
            lgs = Rot(P, [sb("lg%d" % q, [128, 16], F32) for q in range(6)])
            pats = Rot(P, [ps("pat%d" % q, [16, 128], F32) for q in range(2)])
            cbcs = Rot(P, [sb("cbc%d" % q, [128, S], F32) for q in range(2)])
            self.bcast_load(gain_bc[:], self.ffn_norm[i], bgain)
            self.bcast_load(br_bc[:], self.moe_b_router[i], bbr)
            P.dma("sync", lambda h: h.dma_start(out=wr[:], in_=self.moe_w_router[i].rearrange("(k p) e -> p k e", p=128)), writes=[bwr])
            stR = {}

            stRL = {}

            def stage_rl(t):
                xt, bx = xts.next()
                P.dma("sync", lambda h, xt=xt, t=t: h.dma_start(out=xt[:], in_=src[t * 128:(t + 1) * 128, :]), writes=[bx])
                stRL[t] = (xt, bx)

            def stage_r0(t):
                xt, bx = stRL.pop(t)
                junk, bjunk = junks.next()
                stt, bst = stats.next()
                hf, bhf = hfs.next()
                P.op("scalar", lambda h, junk=junk, xt=xt, stt=stt: h.activation(out=junk[:], in_=xt[:], func=AF.Square, accum_out=stt[:, 0:1]),
                     reads=[bx], writes=[bjunk, bst])
                P.op("scalar", lambda h, stt=stt: h.activation(out=stt[:, 1:2], in_=stt[:, 0:1], func=AF.Ln, scale=1.0 / D, bias=self.eps_t[:, 0:1]),
                     reads=[bst, self.b_const], writes=[bst])
                P.op("scalar", lambda h, stt=stt: h.activation(out=stt[:, 2:3], in_=stt[:, 1:2], func=AF.Exp, scale=-0.5), reads=[bst], writes=[bst])
                P.op("vector", lambda h, hf=hf, xt=xt, stt=stt: h.scalar_tensor_tensor(out=hf[:], in0=xt[:], scalar=stt[:, 2:3], in1=gain_bc[:], op0=ALU.mult, op1=ALU.mult),
                     reads=[bx, bst, bgain], writes=[bhf])
                hb, bhb = hbs.next()
                P.op("gpsimd", lambda h, hb=hb, hf=hf: h.tensor_copy(out=hb[:, 0:D], in_=hf[:]), reads=[bhf], uwrites=[bhb])
                stR[t] = dict(stt=stt, bst=bst, hf=hf, bhf=bhf, hb=hb, bhb=bhb)

            def stage_r1(t):
                c = stR[t]
                hf, bhf = c["hf"], c["bhf"]
                hT32, bhT32 = hT32s.next()
                for half in range(2):
                    pTf, bpTf = pTfs.next()
                    for k in range(4):
                        kk = half * 4 + k
                        P.op("tensor", lambda h, pTf=pTf, hf=hf, k=k, kk=kk: h.transpose(out=pTf[:, k, :], in_=hf[:, kk * 128:(kk + 1) * 128], identity=self.ident_f[:]),
                             reads=[bhf, self.b_const], writes=[bpTf])
                    if half == 0:
                        P.op("vector", lambda h, hT32=hT32, pTf=pTf: h.tensor_copy(out=hT32[:, 0:4, :], in_=pTf[:]), reads=[bpTf], uwrites=[bhT32])
                    else:
                        P.op("scalar", lambda h, hT32=hT32, pTf=pTf: h.copy(out=hT32[:, 4:8, :], in_=pTf[:]), reads=[bpTf], uwrites=[bhT32])
                c["hT32"], c["bhT32"] = hT32, bhT32

            def stage_r2(t):
                c = stR[t]
                stt, bst, hb, bhb, hT32, bhT32 = c["stt"], c["bst"], c["hb"], c["bhb"], c["hT32"], c["bhT32"]
                pr, bpr = prs.next()
                for k in range(8):
                    P.op("tensor", lambda h, pr=pr, hT32=hT32, k=k: h.matmul(pr[:], lhsT=hT32[:, k, :], rhs=wr[:, k, :], start=(k == 0), stop=(k == 7)),
                         reads=[bhT32, bwr], writes=[bpr])
                lg, blg = lgs.next()
                P.op("vector", lambda h, lg=lg, pr=pr: h.tensor_tensor(out=lg[:], in0=pr[:], in1=br_bc[:], op=ALU.add), reads=[bpr, bbr], writes=[blg])
                P.op("vector", lambda h, lg=lg, stt=stt: h.reduce_max(out=stt[:, 3:4], in_=lg[:], axis=mybir.AxisListType.X), reads=[blg, bst], writes=[bst])
                P.op("vector", lambda h, stt=stt: h.tensor_scalar(out=stt[:, 4:5], in0=stt[:, 3:4], scalar1=-1.0, scalar2=None, op0=ALU.mult), reads=[bst], writes=[bst])
                P.op("scalar", lambda h, lg=lg, stt=stt: h.activation(out=lg[:], in_=lg[:], func=AF.Exp, bias=stt[:, 4:5], accum_out=stt[:, 5:6]),
                     reads=[blg, bst], writes=[blg, bst])
                P.op("vector", lambda h, stt=stt: h.reciprocal(out=stt[:, 6:7], in_=stt[:, 5:6]), reads=[bst], writes=[bst])
                P.op("vector", lambda h, lg=lg, stt=stt: h.tensor_scalar(out=lg[:], in0=lg[:], scalar1=stt[:, 6:7], scalar2=None, op0=ALU.mult), reads=[blg, bst], writes=[blg])
                P.op("gpsimd", lambda h, hb=hb, lg=lg: h.tensor_copy(out=hb[:, D:1056].bitcast(F32), in_=lg[:]), reads=[blg], uwrites=[bhb])
                P.dma("sync", lambda h, hb=hb, t=t: h.dma_start(out=self.hD[t * 128:(t + 1) * 128, :], in_=hb[:]), reads=[bhb])
                c["lg"], c["blg"] = lg, blg

            def stage_r3(t):
                c = stR.pop(t)
                lg, blg = c["lg"], c["blg"]
                pat, bpat = pats.next()
                P.op("tensor", lambda h, pat=pat, lg=lg: h.transpose(out=pat[:], in_=lg[:], identity=self.ident_f[:]), reads=[blg, self.b_const], writes=[bpat])
                P.op("scalar", lambda h, pat=pat, t=t: h.copy(out=affT[:, t * 128:(t + 1) * 128], in_=pat[:]), reads=[bpat], uwrites=[baffT])

            for step in range(NT + 5):
                if step < NT:
                    stage_rl(step)
                if 0 <= step - 2 < NT:
                    stage_r0(step - 2)
                if 0 <= step - 3 < NT:
                    stage_r1(step - 3)
                if 0 <= step - 4 < NT:
                    stage_r2(step - 4)
                if 0 <= step - 5 < NT:
                    stage_r3(step - 5)
            P.op("vector", lambda h: h.memset(lo[:], 0.0), writes=[blo])
            for it in range(30):
                hstep = 2.0 ** (-(it + 1))
                P.op("vector", lambda h, hstep=hstep: h.tensor_scalar(out=lo[:, 1:2], in0=lo[:, 0:1], scalar1=hstep, scalar2=None, op0=ALU.add), reads=[blo], writes=[blo])
                P.op("vector", lambda h: h.tensor_scalar(out=cjunk[0:16, :], in0=affT[:], scalar1=lo[:, 1:2], scalar2=0.0, op0=ALU.is_ge, op1=ALU.add, accum_out=lo[:, 2:3]),
                     reads=[baffT, blo], writes=[bcj, blo])
                P.op("vector", lambda h, hstep=hstep: h.tensor_scalar(out=lo[:, 3:4], in0=lo[:, 2:3], scalar1=511.5, scalar2=hstep, op0=ALU.is_ge, op1=ALU.mult), reads=[blo], writes=[blo])
                P.op("vector", lambda h: h.tensor_tensor(out=lo[:, 0:1], in0=lo[:, 0:1], in1=lo[:, 3:4], op=ALU.add), reads=[blo], writes=[blo])
            P.op("vector", lambda h: h.tensor_scalar(out=affT[:], in0=affT[:], scalar1=lo[:, 0:1], scalar2=None, op0=ALU.is_ge), reads=[baffT, blo], writes=[baffT])
            P.op("vector", lambda h: h.tensor_tensor_scan(out=affT[:], data0=affT[:], data1=affT[:], initial=0.0, op0=ALU.add, op1=ALU.bypass), reads=[baffT], writes=[baffT])
            bcD = P.buf()
            P.dma("sync", lambda h: h.dma_start(out=self.cD.ap(), in_=affT[:]), reads=[baffT], writes=[bcD])
            P.op("gpsimd", lambda h: h.iota(jvi[:], pattern=[[128, 4]], base=0, channel_multiplier=1), writes=[bjv])
            P.op("vector", lambda h: h.tensor_copy(out=jv[:], in_=jvi[:]), reads=[bjv], writes=[bjv])
            P.op("vector", lambda h: h.tensor_scalar(out=jvh[:], in0=jv[:], scalar1=0.5, scalar2=None, op0=ALU.add), reads=[bjv], writes=[bjv])
            for e in range(16):
                cbc, bcbc = cbcs.next()
                P.dma("sync", lambda h, cbc=cbc, e=e: h.dma_start(out=cbc[:], in_=self.cD[e].partition_broadcast(128)), reads=[bcD], writes=[bcbc])
                for J in range(4):
                    col = e * 4 + J
                    if J < 2:
                        P.op("vector", lambda h, cbc=cbc, J=J, col=col: h.tensor_scalar(
                            out=cjunk[:], in0=cbc[:], scalar1=jv[:, J:J + 1], scalar2=0.0, op0=ALU.is_le, op1=ALU.add, accum_out=idxf[:, col:col + 1]),
                            reads=[bcbc, bjv], writes=[bcj], uwrites=[bidx])
                    else:
                        P.op("scalar", lambda h, cbc=cbc, J=J, col=col: h.activation(
                            out=ajunk[:], in_=cbc[:], func=AF.Sign, scale=-1.0, bias=jvh[:, J:J + 1], accum_out=idxf[:, col:col + 1]),
                            reads=[bcbc, bjv], writes=[baj], uwrites=[bidx])
            iv = idxf[:].rearrange("p (e j) -> p e j", j=4)
            P.op("vector", lambda h: h.tensor_scalar(out=iv[:, :, 2:4], in0=iv[:, :, 2:4], scalar1=0.5, scalar2=2048.0, op0=ALU.mult, op1=ALU.add), reads=[bidx], writes=[bidx])
            P.op("vector", lambda h: h.tensor_copy(out=idxi[:], in_=idxf[:]), reads=[bidx], writes=[bidx])
            P.dma("sync", lambda h: h.dma_start(out=self.idxD.ap(), in_=idxi[:]), reads=[bidx])
            P.end_phase()
        with self.phase() as st:
            sb = lambda n, s, d: st.enter_context(nc.sbuf_tensor("p%d_%s" % (self.pid, n), s, d))
            ps = lambda n, s, d: st.enter_context(nc.psum_tensor("p%d_%s" % (self.pid, n), s, d))
            NS = 8
            ring = [sb("ring%d" % q, [128, 8, 1024], BF16) for q in range(NS)]
            bring = [P.buf() for _ in range(NS)]
            idx = sb("idx", [128, 64], I32); bidx = P.buf()
            xins = Rot(P, [sb("xin%d" % q, [128, 4, 1056], BF16) for q in range(2)])
            xinTs = Rot(P, [sb("xinT%d" % q, [128, 8, 512], BF16) for q in range(1)])
            hidTs = Rot(P, [sb("hidT", [128, 16, 512], BF16)])
            sls = Rot(P, [sb("sl%d" % q, [128, 512], BF16) for q in range(2)])
            ybs = Rot(P, [sb("yb%d" % q, [128, D], F32) for q in range(2)])
            pTs_ = Rot(P, [ps("pTx%d" % q, [128, 8, 128], BF16) for q in range(2)])
            pAs = Rot(P, [ps("pA%d" % q, [128, 512], F32) for q in range(2)])
            pBs = Rot(P, [ps("pB%d" % q, [128, 512], F32) for q in range(2)])
            pYs = Rot(P, [ps("pY%d" % q, [128, 512], F32) for q in range(2)])
            bout = P.buf()
            regbox = {}
            P.dma("sync", lambda h: h.dma_start(out=idx[:], in_=self.idxD.ap()), writes=[bidx])

            def unit_src(e, u):
                if u < 4:
                    w = self.moe_w1 if u % 2 == 0 else self.moe_w3
                    hf = u // 2
                    return w[i, e, :, hf * 1024:(hf + 1) * 1024].rearrange("(k p) f -> p k f", p=128)
                hf = u - 4
                return self.moe_w2[i, e, hf * 1024:(hf + 1) * 1024, :].rearrange("(c p) o -> p c o", p=128)

            def load_unit(e, u):
                s = (6 * e + u) % NS
                self.cast_load(ring[s][:], unit_src(e, u), bring[s])

            def gather(e):
                xin, bxin = xins.next()
                for J in range(4):
                    col = e * 4 + J
                    P.dma("gpsimd", lambda h, xin=xin, J=J, col=col: h.indirect_dma_start(
                        out=xin[:, J, :], out_offset=None, in_=self.hD.ap(),
                        in_offset=bass.IndirectOffsetOnAxis(ap=idx[:, col:col + 1], axis=0)), reads=[bidx], uwrites=[bxin])
                return xin, bxin

            nxt = gather(0)
            for u in range(6):
                load_unit(0, u)
            for e in range(16):
                xin, bxin = nxt
                if e + 1 < 16:
                    nxt = gather(e + 1)
                xinT, bxT = xinTs.next()
                for J in range(4):
                    pT_, bpT_ = pTs_.next()
                    for k in range(8):
                        P.op("tensor", lambda h, pT_=pT_, xin=xin, J=J, k=k: h.transpose(out=pT_[:, k, :], in_=xin[:, J, k * 128:(k + 1) * 128], identity=self.ident_bf[:]),
                             reads=[bxin, self.b_const], writes=[bpT_])
                    if J % 2 == 0:
                        P.op("vector", lambda h, pT_=pT_, xinT=xinT, J=J: h.tensor_copy(out=xinT[:, :, J * 128:(J + 1) * 128], in_=pT_[:]), reads=[bpT_], uwrites=[bxT])
                    else:
                        P.op("scalar", lambda h, pT_=pT_, xinT=xinT, J=J: h.copy(out=xinT[:, :, J * 128:(J + 1) * 128], in_=pT_[:]), reads=[bpT_], uwrites=[bxT])
                hidT, bhid = hidTs.next()
                for hf in range(2):
                    s1 = (6 * e + 2 * hf) % NS
                    s3 = (6 * e + 2 * hf + 1) % NS
                    for fcl in range(8):
                        fc = hf * 8 + fcl
                        pA, bpA = pAs.next()
                        pB, bpB = pBs.next()
                        for k in range(8):
                            P.op("tensor", lambda h, pA=pA, s1=s1, k=k, fcl=fcl, xinT=xinT: h.matmul(
                                pA[:], lhsT=ring[s1][:, k, fcl * 128:(fcl + 1) * 128], rhs=xinT[:, k, :], start=(k == 0), stop=(k == 7)),
                                reads=[bring[s1], bxT], writes=[bpA])
                        for k in range(8):
                            P.op("tensor", lambda h, pB=pB, s3=s3, k=k, fcl=fcl, xinT=xinT: h.matmul(
                                pB[:], lhsT=ring[s3][:, k, fcl * 128:(fcl + 1) * 128], rhs=xinT[:, k, :], start=(k == 0), stop=(k == 7)),
                                reads=[bring[s3], bxT], writes=[bpB])
                        sl, bsl = sls.next()
                        P.op("scalar", lambda h, sl=sl, pA=pA: h.activation(out=sl[:], in_=pA[:], func=AF.Silu), reads=[bpA], writes=[bsl])
                        P.op("vector", lambda h, sl=sl, pB=pB, hidT=hidT, fc=fc: h.tensor_tensor(out=hidT[:, fc, :], in0=pB[:], in1=sl[:], op=ALU.mult),
                             reads=[bpB, bsl], uwrites=[bhid])
                    if e + 1 < 16:
                        if hf == 0:
                            load_unit(e + 1, 0)
                            load_unit(e + 1, 1)
                            load_unit(e + 1, 2)
                            load_unit(e + 1, 3)
                        else:
                            load_unit(e + 1, 4)
                            load_unit(e + 1, 5)
                sA = (6 * e + 4) % NS
                sB = (6 * e + 5) % NS
                for J in range(4):
                    yb, byb = ybs.next()
                    for ob in range(2):
                        pY, bpY = pYs.next()
                        for fc in range(16):
                            sw = sA if fc < 8 else sB
                            P.op("tensor", lambda h, pY=pY, hidT=hidT, fc=fc, J=J, ob=ob, sw=sw: h.matmul(
                                pY[:], lhsT=hidT[:, fc, J * 128:(J + 1) * 128], rhs=ring[sw][:, fc % 8, ob * 512:(ob + 1) * 512], start=(fc == 0), stop=(fc == 15)),
                                reads=[bhid, bring[sw]], writes=[bpY])
                        gate = xin[:, J, D:1056].bitcast(F32)[:, e:e + 1]
                        if ob == 0:
                            P.op("vector", lambda h, yb=yb, pY=pY, gate=gate, ob=ob: h.tensor_scalar(out=yb[:, ob * 512:(ob + 1) * 512], in0=pY[:], scalar1=gate, scalar2=None, op0=ALU.mult),
                                 reads=[bpY, bxin], uwrites=[byb])
                        else:
                            P.op("scalar", lambda h, yb=yb, pY=pY, gate=gate, ob=ob: h.activation(out=yb[:, ob * 512:(ob + 1) * 512], in_=pY[:], func=AF.Copy, scale=gate),
                                 reads=[bpY, bxin], uwrites=[byb])
                    col = e * 4 + J
                    def scat(h, yb=yb, col=col):
                        if "r" not in regbox:
                            regbox["r"] = h.to_reg(S - 1)
                        return h.indirect_dma_start(
                            out=self.out.ap(), out_offset=bass.IndirectOffsetOnAxis(ap=idx[:, col:col + 1], axis=0), in_=yb[:], in_offset=None,
                            bounds_check=regbox["r"], oob_is_err=True, compute_op=ALU.add)
                    P.dma("gpsimd", scat, reads=[byb, bidx], writes=[bout])
            P.end_phase()

    def build(self):
        P = self.P
        self.consts()
        first = True
        for i in self.layers:
            if "mix" in self.sub:
                src = self.x_in if first else self.out
                if i % 2 == 0:
                    self.gmlp(i, src)
                else:
                    self.attn(i, src)
                first = False
            if "moe" in self.sub:
                if first:
                    with self.phase() as st:
                        t = st.enter_context(self.nc.sbuf_tensor("cp%d" % self.pid, [128, 8, D], F32))
                        b = P.buf()
                        for q in range(4):
                            P.dma("sync", lambda h, q=q: h.dma_start(out=t[:], in_=self.x_in[q * 1024:(q + 1) * 1024, :].rearrange("(a p) d -> p a d", p=128)), writes=[b])
                            P.dma("sync", lambda h, q=q: h.dma_start(out=self.out[q * 1024:(q + 1) * 1024, :].rearrange("(a p) d -> p a d", p=128), in_=t[:]), reads=[b])
                        P.end_phase()
                    first = False
                self.moe(i)


_WNAMES = ["mix_norm", "ffn_norm", "gm_w_in", "gm_b_in", "gm_v_norm", "gm_w_s", "gm_b_s", "gm_w_out", "gm_b_out",
           "at_w_qkv", "at_q_norm", "at_k_norm", "at_w_o", "moe_w_router", "moe_b_router", "moe_w1", "moe_w3", "moe_w2"]


def build_nc(layers=(0, 1, 2, 3), sub=("mix", "moe")):
    nc = bass.Bass("TRN2", target_bir_lowering=False)
    with contextlib.ExitStack() as stack:
        k = K(nc, stack, layers, sub)
        k.build()
    return nc


def kernel(**inputs):
    x = np.ascontiguousarray(inputs["x"], dtype=np.float32)
    B = x.shape[0]
    w = {n: np.ascontiguousarray(inputs[n], dtype=np.float32) for n in _WNAMES}
    nc = build_nc()
    in_maps = []
    for b in range(B):
        m = {"x": x[b]}
        m.update(w)
        in_maps.append(m)
    res = run_bass_kernel_spmd(nc, in_maps, core_ids=list(range(B)))
    return np.stack([np.asarray(r["out"]) for r in res.results], axis=0).astype(np.float32)
```

```python
import contextlib
import numpy as np
import concourse.bass as bass
import concourse.mybir as mybir
from concourse.bass_utils import run_bass_kernel_spmd

F32 = mybir.dt.float32
BF16 = mybir.dt.bfloat16
I32 = mybir.dt.int32
AF = mybir.ActivationFunctionType
ALU = mybir.AluOpType
GELU = AF.Gelu_apprx_tanh

S = 4096
D = 1024
NT = S // 128
EPS = 1e-6
ENGS = ("sync", "scalar", "vector", "gpsimd", "tensor")


class Buf:
    __slots__ = ("name", "writers", "readers", "unord", "pre")

    def __init__(self, name="b"):
        self.name = name
        self.writers = []
        self.readers = []
        self.unord = False
        self.pre = []


class Op:
    __slots__ = ("eng", "fn", "deps", "is_dma", "signal", "sigval", "sem", "idx")

    def __init__(self, eng, fn, is_dma):
        self.eng = eng
        self.fn = fn
        self.deps = []
        self.is_dma = is_dma
        self.signal = False
        self.sigval = None
        self.sem = None
        self.idx = None


class Prog:
    def __init__(self, nc, stack, n_dma_sems=64):
        self.nc = nc
        self.n_dma_sems = n_dma_sems
        self.esem = {e: stack.enter_context(nc.semaphore("es_" + e)) for e in ENGS}
        self.dsem = [stack.enter_context(nc.semaphore("ds_%d" % i)) for i in range(n_dma_sems)]
        self.ecount = {e: 0 for e in ENGS}
        self.dcount = [0] * n_dma_sems
        self.seen = {e: {} for e in ENGS}
        self.bufs = []
        self.nops = 0
        self._reset_phase()

    def _reset_phase(self):
        self.ops = {e: [] for e in ENGS}
        self.dma_rr = 0
        self.dma_last = [None] * self.n_dma_sems
        self.last_compute = {e: None for e in ENGS}
        for b in self.bufs:
            b.writers = []
            b.readers = []
            b.unord = False
            b.pre = []

    def buf(self, name="b"):
        b = Buf(name)
        self.bufs.append(b)
        return b

    def _add(self, eng, fn, reads, writes, uwrites, is_dma):
        op = Op(eng, fn, is_dma)
        op.idx = self.nops
        self.nops += 1
        deps = []
        for b in reads:
            deps.extend(b.writers)
        for b in writes:
            deps.extend(b.writers)
            deps.extend(b.readers)
        for b in uwrites:
            if not (b.unord and not b.readers):
                b.pre = list(b.writers) + list(b.readers)
                b.writers = []
                b.readers = []
                b.unord = True
            deps.extend(b.pre)
        if is_dma:
            s = self.dma_rr
            self.dma_rr = (self.dma_rr + 1) % self.n_dma_sems
            op.sem = s
            prev = self.dma_last[s]
            if prev is not None:
                deps.append(prev)
            self.dma_last[s] = op
        seen = set()
        for d in deps:
            if d is op or id(d) in seen:
                continue
            seen.add(id(d))
            if (not d.is_dma) and d.eng == "tensor" and eng == "tensor" and not is_dma:
                continue
            op.deps.append(d)
            if not d.is_dma:
                d.signal = True
        for b in reads:
            b.readers.append(op)
        for b in writes:
            b.writers = [op]
            b.readers = []
            b.unord = False
        for b in uwrites:
            b.writers.append(op)
        self.ops[eng].append(op)
        if not is_dma:
            self.last_compute[eng] = op
        return op

    def op(self, eng, fn, reads=(), writes=(), uwrites=()):
        return self._add(eng, fn, reads, writes, uwrites, False)

    def dma(self, eng, fn, reads=(), writes=(), uwrites=()):
        return self._add(eng, fn, reads, writes, uwrites, True)

    def end_phase(self):
        deps = [o for o in self.last_compute.values() if o is not None]
        deps += [o for o in self.dma_last if o is not None]
        for d in deps:
            if not d.is_dma:
                d.signal = True
        for e in ENGS:
            op = Op(e, None, False)
            op.idx = self.nops
            self.nops += 1
            op.deps = list(deps)
            self.ops[e].append(op)
        for e in ENGS:
            c = self.ecount[e]
            for o in self.ops[e]:
                if (not o.is_dma) and o.signal:
                    c += 1
                    o.sigval = c
            self.ecount[e] = c
        allops = []
        for e in ENGS:
            allops.extend(self.ops[e])
        allops.sort(key=lambda o: o.idx)
        for o in allops:
            if o.is_dma:
                self.dcount[o.sem] += 16
                o.sigval = self.dcount[o.sem]

        def run(e, h):
            seen = self.seen[e]
            for o in self.ops[e]:
                for d in o.deps:
                    if d.is_dma:
                        key = ("d", d.sem)
                        s = self.dsem[d.sem]
                    else:
                        if d.eng == e and d.fn is None:
                            continue
                        key = ("e", d.eng)
                        s = self.esem[d.eng]
                    if seen.get(key, 0) >= d.sigval:
                        continue
                    seen[key] = d.sigval
                    h.wait_ge(s, d.sigval)
                if o.fn is None:
                    continue
                ins = o.fn(h)
                if o.is_dma:
                    ins.then_inc(self.dsem[o.sem], 16)
                elif o.signal:
                    ins.then_inc(self.esem[e], 1)

        with self.nc.Block() as block:
            @block.sync
            def _(h):
                run("sync", h)

            @block.scalar
            def _(h):
                run("scalar", h)

            @block.vector
            def _(h):
                run("vector", h)

            @block.gpsimd
            def _(h):
                run("gpsimd", h)

            @block.tensor
            def _(h):
                run("tensor", h)
        self._reset_phase()


class Rot:
    def __init__(self, P, tiles):
        self.tiles = tiles
        self.bufs = [P.buf() for _ in tiles]
        self.i = 0

    def next(self):
        t, b = self.tiles[self.i], self.bufs[self.i]
        self.i = (self.i + 1) % len(self.tiles)
        return t, b


class K:
    def __init__(self, nc, stack, layers=(0, 1, 2, 3), sub=("mix", "moe")):
        self.nc = nc
        self.pid = 0
        self.P = Prog(nc, stack)
        self.layers = layers
        self.sub = sub
        dt = nc.dram_tensor
        self.x_in = dt("x", [S, D], F32, kind="ExternalInput")
        self.mix_norm = dt("mix_norm", [4, D], F32, kind="ExternalInput")
        self.ffn_norm = dt("ffn_norm", [4, D], F32, kind="ExternalInput")
        self.gm_w_in = dt("gm_w_in", [2, D, 6144], F32, kind="ExternalInput")
        self.gm_b_in = dt("gm_b_in", [2, 6144], F32, kind="ExternalInput")
        self.gm_v_norm = dt("gm_v_norm", [2, 3072], F32, kind="ExternalInput")
        self.gm_w_s = dt("gm_w_s", [2, 8, 128, 128], F32, kind="ExternalInput")
        self.gm_b_s = dt("gm_b_s", [2, 8, 128], F32, kind="ExternalInput")
        self.gm_w_out = dt("gm_w_out", [2, 3072, D], F32, kind="ExternalInput")
        self.gm_b_out = dt("gm_b_out", [2, D], F32, kind="ExternalInput")
        self.at_w_qkv = dt("at_w_qkv", [2, D, 4608], F32, kind="ExternalInput")
        self.at_q_norm = dt("at_q_norm", [2, 3, 64], F32, kind="ExternalInput")
        self.at_k_norm = dt("at_k_norm", [2, 3, 64], F32, kind="ExternalInput")
        self.at_w_o = dt("at_w_o", [2, 512, D], F32, kind="ExternalInput")
        self.moe_w_router = dt("moe_w_router", [4, D, 16], F32, kind="ExternalInput")
        self.moe_b_router = dt("moe_b_router", [4, 16], F32, kind="ExternalInput")
        self.moe_w1 = dt("moe_w1", [4, 16, D, 2048], F32, kind="ExternalInput")
        self.moe_w3 = dt("moe_w3", [4, 16, D, 2048], F32, kind="ExternalInput")
        self.moe_w2 = dt("moe_w2", [4, 16, 2048, D], F32, kind="ExternalInput")
        self.out = dt("out", [S, D], F32, kind="ExternalOutput")
        self.hT_D = dt("hT_D", [NT, 128, 8, 128], BF16)
        self.sT_D = dt("sT_D", [NT, 128, 24, 128], BF16)
        self.N_D = [dt("N_D%d" % g, [S, 520], F32) for g in range(3)]
        self.hD = dt("hD", [S, 1056], BF16)
        self.cD = dt("cD", [16, S], F32)
        self.idxD = dt("idxD", [128, 64], I32)
        sb = lambda n, s, d: stack.enter_context(nc.sbuf_tensor("p%d_%s" % (self.pid, n), s, d))
        self.ident_bf = sb("ident_bf", [128, 128], BF16)
        self.ident_f = sb("ident_f", [128, 128], F32)
        self.blockones = sb("blockones", [128, 128], BF16)
        self.b_const = self.P.buf("const")
        self.pid = 0

    def consts(self):
        P = self.P
        ib, if_, bo, bc = self.ident_bf, self.ident_f, self.blockones, self.b_const
        P.op("gpsimd", lambda h: h.memset(if_[:], 1.0), writes=[bc])
        P.op("gpsimd", lambda h: h.affine_select(out=if_[:], in_=if_[:], pattern=[[-1, 128]],
                                                  compare_op=ALU.is_equal, fill=0.0, base=0,
                                                  channel_multiplier=1), reads=[bc], writes=[bc])
        P.op("vector", lambda h: h.tensor_copy(out=ib[:], in_=if_[:]), reads=[bc], writes=[bc])
        P.op("vector", lambda h: h.memset(bo[:], 0.0), reads=[bc], writes=[bc])
        P.op("vector", lambda h: h.memset(bo[0:64, 0:64], 1.0), reads=[bc], writes=[bc])
        P.op("vector", lambda h: h.memset(bo[64:128, 64:128], 1.0), reads=[bc], writes=[bc])

    def cast_load(self, dst, src, wbuf, max_bytes=4 << 20):
        P = self.P
        _, kk, n = dst.shape
        ncol = n
        while ncol > 2048:
            ncol //= 2
        kstep = max(1, min(kk, max_bytes // (128 * ncol * 4)))
        for k0 in range(0, kk, kstep):
            k1 = min(kk, k0 + kstep)
            for c0 in range(0, n, ncol):
                P.dma("gpsimd", lambda h, a=dst[:, k0:k1, c0:c0 + ncol], b=src[:, k0:k1, c0:c0 + ncol]:
                      h.dma_start(out=a, in_=b), uwrites=[wbuf])

    def bcast_load(self, dst, src_row, wbuf, eng="sync"):
        self.P.dma(eng, lambda h: h.dma_start(out=dst, in_=src_row.partition_broadcast(128)), writes=[wbuf])

    def rows_ap(self, t, ncols, start, step, nrows=128):
        return bass.AP(t, start * ncols, [[step * ncols, nrows], [1, ncols]])

    def norm_tile(self, xt, bx, gain_bc, bgain, W, hb_dtype=BF16):
        P = self.P
        junk, bjunk = W["junk"].next()
        st, bst = W["stat"].next()
        hb, bhb = W["hb"].next()
        P.op("scalar", lambda h: h.activation(out=junk[:, 0:D], in_=xt, func=AF.Square, accum_out=st[:, 0:1]),
             reads=[bx], writes=[bjunk, bst])
        P.op("scalar", lambda h: h.activation(out=st[:, 1:2], in_=st[:, 0:1], func=AF.Ln, scale=1.0 / D, bias=self.eps_t[:, 0:1]),
             reads=[bst, self.b_const], writes=[bst])
        P.op("scalar", lambda h: h.activation(out=st[:, 2:3], in_=st[:, 1:2], func=AF.Exp, scale=-0.5), reads=[bst], writes=[bst])
        P.op("vector", lambda h: h.scalar_tensor_tensor(out=hb[:], in0=xt, scalar=st[:, 2:3], in1=gain_bc[:],
                                                        op0=ALU.mult, op1=ALU.mult),
             reads=[bx, bst, bgain], writes=[bhb])
        return hb, bhb

    def transpose8(self, hb, bhb, dst, bdst, W, eng="vector"):
        P = self.P
        pT, bpT = W["pT"].next()
        for k in range(8):
            P.op("tensor", lambda h, k=k: h.transpose(out=pT[:, k, :], in_=hb[:, k * 128:(k + 1) * 128],
                                                      identity=self.ident_bf[:]),
                 reads=[bhb, self.b_const], writes=[bpT])
        if eng == "vector":
            P.op("vector", lambda h: h.tensor_copy(out=dst, in_=pT[:]), reads=[bpT], writes=[bdst])
        else:
            P.op("scalar", lambda h: h.copy(out=dst, in_=pT[:]), reads=[bpT], writes=[bdst])

    def phase(self):
        self.pid += 1
        return contextlib.ExitStack()

    def gmlp(self, i, src):
        nc, P = self.nc, self.P
        j = i // 2
        with self.phase() as st:
            sb = lambda n, s, d: st.enter_context(nc.sbuf_tensor("p%d_%s" % (self.pid, n), s, d))
            ps = lambda n, s, d: st.enter_context(nc.psum_tensor("p%d_%s" % (self.pid, n), s, d))
            wv = sb("wv", [128, 8, 3072], BF16); bwv = [P.buf() for _ in range(6)]
            gain_bc = sb("gain_bc", [128, D], F32); bgain = P.buf()
            bv_bc = sb("bv_bc", [128, 3072], F32); bbv = P.buf()
            wsf = sb("wsf", [128, 8, 128], F32); wsb = sb("wsb", [128, 8, 128], BF16)
            wsT = sb("wsT", [128, 8, 128], BF16); bws = P.buf()
            vgT = sb("vgT", [128, 24], F32); bvg = P.buf()
            bs_bc = sb("bs_bc", [128, 8, 128], F32); bbs = P.buf()
            self.eps_t = sb("eps_t", [128, 1], F32)
            P.op("vector", lambda h: h.memset(self.eps_t[:], EPS), reads=[self.b_const], writes=[self.b_const])
            W = {
                "junk": Rot(P, [sb("junk", [128, 3072], BF16)]),
                "stat": Rot(P, [sb("stat%d" % q, [128, 4], F32) for q in range(4)]),
                "hb": Rot(P, [sb("hb%d" % q, [128, D], BF16) for q in range(3)]),
                "pT": Rot(P, [ps("pT%d" % q, [128, 8, 128], BF16) for q in range(2)]),
            }
            xts = Rot(P, [sb("xt%d" % q, [128, D], F32) for q in range(4)])
            hTs = Rot(P, [sb("hT%d" % q, [128, 8, 128], BF16) for q in range(3)])
            pvs = Rot(P, [ps("pv%d" % q, [128, 512], F32) for q in range(3)])
            tmps = Rot(P, [sb("tmp%d" % q, [128, 512], F32) for q in range(4)])
            vs = Rot(P, [sb("v%d" % q, [128, 3072], BF16) for q in range(3)])
            vns = Rot(P, [sb("vn%d" % q, [128, 3072], BF16) for q in range(3)])
            pss = Rot(P, [ps("pss%d" % q, [128, 4, 128], F32) for q in range(3)])
            sTs = Rot(P, [sb("sT%d" % q, [128, 24, 128], BF16) for q in range(3)])
            stv = Rot(P, [sb("stv%d" % q, [128, 4], F32) for q in range(4)])
            self.bcast_load(gain_bc[:], self.mix_norm[i], bgain)
            self.bcast_load(bv_bc[:], self.gm_b_in[j, 3072:6144], bbv)
            self.bcast_load(bs_bc[:].rearrange("p g t -> p (g t)"), self.gm_b_s[j].rearrange("g t -> (g t)"), bbs)
            P.dma("sync", lambda h: h.dma_start(out=vgT[:], in_=self.gm_v_norm[j].rearrange("(c p) -> p c", p=128),
                                                allow_slow_non_contiguous=True), writes=[bvg])
            P.dma("sync", lambda h: h.dma_start(out=wsf[:], in_=self.gm_w_s[j].rearrange("g t s -> t g s")), writes=[bws])
            wvsrc = self.gm_w_in[j, :, 3072:6144].rearrange("(k p) f -> p k f", p=128)
            for blk in range(6):
                self.cast_load(wv[:, :, blk * 512:(blk + 1) * 512], wvsrc[:, :, blk * 512:(blk + 1) * 512], bwv[blk])
            P.op("vector", lambda h: h.tensor_copy(out=wsb[:], in_=wsf[:]), reads=[bws], writes=[bws])
            pT0, bpT0 = W["pT"].next()
            for g in range(8):
                P.op("tensor", lambda h, g=g: h.transpose(out=pT0[:, g, :], in_=wsb[:, g, :], identity=self.ident_bf[:]),
                     reads=[bws, self.b_const], writes=[bpT0])
            P.op("vector", lambda h: h.tensor_copy(out=wsT[:], in_=pT0[:]), reads=[bpT0, bws], writes=[bws])
            stV = {}

            stL = {}

            def stage_l(t):
                xt, bx = xts.next()
                P.dma("sync", lambda h, xt=xt, t=t: h.dma_start(out=xt[:], in_=src[t * 128:(t + 1) * 128, :]), writes=[bx])
                stL[t] = (xt, bx)

            def stage_n(t):
                xt, bx = stL.pop(t)
                hb, bhb = self.norm_tile(xt[:], bx, gain_bc, bgain, W)
                hT, bhT = hTs.next()
                self.transpose8(hb, bhb, hT[:], bhT, W)
                P.dma("sync", lambda h, hT=hT, t=t: h.dma_start(out=self.hT_D[t], in_=hT[:]), reads=[bhT])
                stV[t] = [hT, bhT]

            def stage_m(t):
                hT, bhT = stV[t]
                v, bv = vs.next()
                for blk in range(6):
                    pv, bpv = pvs.next()
                    for k in range(8):
                        P.op("tensor", lambda h, pv=pv, hT=hT, k=k, blk=blk: h.matmul(
                            pv[:], lhsT=hT[:, k, :], rhs=wv[:, k, blk * 512:(blk + 1) * 512], start=(k == 0), stop=(k == 7)),
                            reads=[bhT, bwv[blk]], writes=[bpv])
                    tmp, btmp = tmps.next()
                    P.op("vector", lambda h, tmp=tmp, pv=pv, blk=blk: h.tensor_tensor(
                        out=tmp[:], in0=pv[:], in1=bv_bc[:, blk * 512:(blk + 1) * 512], op=ALU.add),
                        reads=[bpv, bbv], writes=[btmp])
                    P.op("scalar", lambda h, tmp=tmp, v=v, blk=blk: h.activation(
                        out=v[:, blk * 512:(blk + 1) * 512], in_=tmp[:], func=GELU), reads=[btmp], writes=[bv])
                stV[t] = [v, bv]

            def stage_bs(t):
                v, bv = stV[t]
                junk, bjunk = W["junk"].next()
                sv, bsv = stv.next()
                P.op("scalar", lambda h, junk=junk, v=v, sv=sv: h.activation(out=junk[:], in_=v[:], func=AF.Square, accum_out=sv[:, 0:1]),
                     reads=[bv], writes=[bjunk, bsv])
                P.op("scalar", lambda h, sv=sv: h.activation(out=sv[:, 1:2], in_=sv[:, 0:1], func=AF.Ln, scale=1.0 / 3072, bias=self.eps_t[:, 0:1]),
                     reads=[bsv, self.b_const], writes=[bsv])
                P.op("scalar", lambda h, sv=sv: h.activation(out=sv[:, 2:3], in_=sv[:, 1:2], func=AF.Exp, scale=-0.5), reads=[bsv], writes=[bsv])
                vn, bvn = vns.next()
                P.op("vector", lambda h, vn=vn, v=v, sv=sv: h.tensor_scalar(out=vn[:], in0=v[:], scalar1=sv[:, 2:3], scalar2=None, op0=ALU.mult),
                     reads=[bv, bsv], writes=[bvn])
                stV[t] = [vn, bvn]

            def stage_bm(t):
                vn, bvn = stV.pop(t)
                sT, bsT = sTs.next()
                for q in range(6):
                    pq, bpq = pss.next()
                    for m in range(4):
                        fc = q * 4 + m
                        P.op("tensor", lambda h, pq=pq, vn=vn, fc=fc, m=m: h.matmul(
                            pq[:, m, :], lhsT=vn[:, fc * 128:(fc + 1) * 128], rhs=wsT[:, fc // 3, :], start=True, stop=True),
                            reads=[bvn, bws], writes=[bpq])
                    for m in range(4):
                        fc = q * 4 + m
                        P.op("vector", lambda h, pq=pq, sT=sT, fc=fc, m=m: h.scalar_tensor_tensor(
                            out=sT[:, fc, :], in0=pq[:, m, :], scalar=vgT[:, fc:fc + 1], in1=bs_bc[:, fc // 3, :],
                            op0=ALU.mult, op1=ALU.add), reads=[bpq, bvg, bbs], writes=[bsT])
                P.dma("sync", lambda h, sT=sT, t=t: h.dma_start(out=self.sT_D[t], in_=sT[:]), reads=[bsT])

            stage_l(0)
            stage_l(1)
            stage_l(2)
            stage_n(0)
            stage_m(0)
            stage_bs(0)
            stage_n(1)
            for t in range(NT):
                if t + 3 < NT:
                    stage_l(t + 3)
                if t + 1 < NT:
                    stage_m(t + 1)
                stage_bm(t)
                if t + 1 < NT:
                    stage_bs(t + 1)
                if t + 2 < NT:
                    stage_n(t + 2)
            P.end_phase()
        with self.phase() as st:
            sb = lambda n, s, d: st.enter_context(nc.sbuf_tensor("p%d_%s" % (self.pid, n), s, d))
            ps = lambda n, s, d: st.enter_context(nc.psum_tensor("p%d_%s" % (self.pid, n), s, d))
            wu = sb("wu", [128, 8, 3072], BF16); bwu = [P.buf() for _ in range(6)]
            wo = sb("wo", [128, 24, D], BF16); bwo = [P.buf() for _ in range(4)]
            buT = sb("buT", [128, 24], F32); bbu = P.buf()
            bout_bc = sb("bout_bc", [128, D], F32); bbo = P.buf()
            hTss = Rot(P, [sb("hTs%d" % q, [128, 2, 8, 128], BF16) for q in range(2)])
            sTss = Rot(P, [sb("sTs%d" % q, [128, 2, 24, 128], BF16) for q in range(2)])
            xss = Rot(P, [sb("xs%d" % q, [128, 2, D], F32) for q in range(2)])
            xns = Rot(P, [sb("xn%d" % q, [128, 2, D], F32) for q in range(2)])
            gTs = Rot(P, [sb("gT", [128, 24, 2, 128], BF16)])
            uts = Rot(P, [sb("ut%d" % q, [128, 2, 128], BF16) for q in range(2)])
            pus = Rot(P, [ps("pu%d" % q, [128, 2, 128], F32) for q in range(3)])
            pos = Rot(P, [ps("po%d" % q, [128, 512], F32) for q in range(2)])
            self.bcast_load(bout_bc[:], self.gm_b_out[j], bbo)
            P.dma("sync", lambda h: h.dma_start(out=buT[:], in_=self.gm_b_in[j, 0:3072].rearrange("(c p) -> p c", p=128),
                                                allow_slow_non_contiguous=True), writes=[bbu])
            wusrc = self.gm_w_in[j, :, 0:3072].rearrange("(k p) f -> p k f", p=128)
            wosrc = self.gm_w_out[j].rearrange("(c p) o -> p c o", p=128)
            for blk in range(6):
                self.cast_load(wu[:, :, blk * 512:(blk + 1) * 512], wusrc[:, :, blk * 512:(blk + 1) * 512], bwu[blk])
            for blk in range(4):
                self.cast_load(wo[:, blk * 6:(blk + 1) * 6, :], wosrc[:, blk * 6:(blk + 1) * 6, :], bwo[blk])
            stU = {}

            def load_u(s):
                hTs_, bh = hTss.next()
                sTs_, bs_ = sTss.next()
                xs, bxs = xss.next()
                P.dma("sync", lambda h, a=hTs_, s=s: h.dma_start(out=a[:], in_=self.hT_D[2 * s:2 * s + 2].rearrange("t p k n -> p t k n")), writes=[bh])
                P.dma("sync", lambda h, a=sTs_, s=s: h.dma_start(out=a[:], in_=self.sT_D[2 * s:2 * s + 2].rearrange("t p c n -> p t c n")), writes=[bs_])
                P.dma("sync", lambda h, a=xs, s=s: h.dma_start(out=a[:], in_=src[s * 256:(s + 1) * 256, :].rearrange("(t p) d -> p t d", p=128)), writes=[bxs])
                stU[s] = (hTs_, bh, sTs_, bs_, xs, bxs)

            load_u(0)
            for s in range(NT // 2):
                if s + 1 < NT // 2:
                    load_u(s + 1)
                hTs_, bh, sTs_, bs_, xs, bxs = stU.pop(s)
                for t in range(2):
                    P.op("gpsimd", lambda h, xs=xs, t=t: h.tensor_tensor(out=xs[:, t, :], in0=xs[:, t, :], in1=bout_bc[:], op=ALU.add),
                         reads=[bxs, bbo], writes=[bxs])
                gT, bgT = gTs.next()
                for fc in range(24):
                    pu, bpu = pus.next()
                    for k in range(8):
                        P.op("tensor", lambda h, pu=pu, a=hTs_, k=k, fc=fc: h.matmul(
                            pu[:], lhsT=wu[:, k, fc * 128:(fc + 1) * 128], rhs=a[:, :, k, :], start=(k == 0), stop=(k == 7)),
                            reads=[bwu[fc // 4], bh], writes=[bpu])
                    ut, but = uts.next()
                    P.op("scalar", lambda h, pu=pu, ut=ut, fc=fc: h.activation(out=ut[:], in_=pu[:], func=GELU, bias=buT[:, fc:fc + 1]),
                         reads=[bpu, bbu], writes=[but])
                    P.op("vector", lambda h, ut=ut, gT=gT, a=sTs_, fc=fc: h.tensor_tensor(
                        out=gT[:, fc, :, :], in0=ut[:], in1=a[:, :, fc, :], op=ALU.mult), reads=[but, bs_], writes=[bgT])
                xn, bxn = xns.next()
                for t in range(2):
                    for ob in range(2):
                        po, bpo = pos.next()
                        for fc in range(24):
                            P.op("tensor", lambda h, po=po, gT=gT, fc=fc, t=t, ob=ob: h.matmul(
                                po[:], lhsT=gT[:, fc, t, :], rhs=wo[:, fc, ob * 512:(ob + 1) * 512], start=(fc == 0), stop=(fc == 23)),
                                reads=[bgT, bwo[fc // 6]], writes=[bpo])
                        P.op("vector", lambda h, po=po, xn=xn, xs=xs, t=t, ob=ob: h.tensor_tensor(
                            out=xn[:, t, ob * 512:(ob + 1) * 512], in0=po[:], in1=xs[:, t, ob * 512:(ob + 1) * 512], op=ALU.add),
                            reads=[bpo, bxs], writes=[bxn])
                P.dma("sync", lambda h, xn=xn, s=s: h.dma_start(out=self.out[s * 256:(s + 1) * 256, :].rearrange("(t p) d -> p t d", p=128), in_=xn[:]),
                      reads=[bxn])
            P.end_phase()

    def attn(self, i, src):
        nc, P = self.nc, self.P
        j = i // 2
        dils = (1, 4, 16)
        with self.phase() as st:
            sb = lambda n, s, d: st.enter_context(nc.sbuf_tensor("p%d_%s" % (self.pid, n), s, d))
            ps = lambda n, s, d: st.enter_context(nc.psum_tensor("p%d_%s" % (self.pid, n), s, d))
            gain_bc = sb("gain_bc", [128, D], F32); bgain = P.buf()
            self.eps_t = sb("eps_t", [128, 1], F32)
            P.op("vector", lambda h: h.memset(self.eps_t[:], EPS), reads=[self.b_const], writes=[self.b_const])
            wq = sb("wq", [128, 8, 512], BF16); wk = sb("wk", [128, 8, 512], BF16); wvv = sb("wvv", [128, 8, 512], BF16)
            bwq, bwk, bwvv = P.buf(), P.buf(), P.buf()
            gq = sb("gq", [128, 1], F32); gk = sb("gk", [128, 1], F32); bg = P.buf()
            QT = sb("QT", [128, 4, S], BF16); KT = sb("KT", [128, 4, S], BF16)
            V = sb("V", [128, NT, 8, 65], BF16)
            bQT, bKT, bV = P.buf(), P.buf(), P.buf()
            E = sb("E", [128, 3, 8, 128], BF16); bE = P.buf()
            rel = sb("rel", [128, 3, 128], F32); reli = sb("reli", [128, 3, 128], I32)
            msk = sb("msk", [128, 3, 128], F32); etmp = sb("etmp", [128, 3, 128], F32); brel = P.buf()
            W = {
                "junk": Rot(P, [sb("junk", [128, D], BF16)]),
                "stat": Rot(P, [sb("stat%d" % q, [128, 4], F32) for q in range(4)]),
                "hb": Rot(P, [sb("hb%d" % q, [128, D], BF16) for q in range(3)]),
                "pT": Rot(P, [ps("pT%d" % q, [128, 8, 128], BF16) for q in range(1)]),
            }
            xts = Rot(P, [sb("xt%d" % q, [128, D], F32) for q in range(3)])
            hTbs = Rot(P, [sb("hTb%d" % q, [128, 8, 512], BF16) for q in range(2)])
            pqs = Rot(P, [ps("pq%d" % q, [128, 512], F32) for q in range(4)])
            pns = Rot(P, [ps("pn%d" % q, [128, 512], F32) for q in range(1)])
            sqs = Rot(P, [sb("sq%d" % q, [128, 512], BF16) for q in range(3)])
            stds = Rot(P, [sb("std%d" % q, [128, 512], F32) for q in range(3)])
            pSs = pqs
            pOs = [ps("pO%d" % q, [128, 4, 65], F32) for q in range(2)]
            bpOs = [P.buf(), P.buf()]
            exs = Rot(P, [sb("ex%d" % q, [128, 4, 128], BF16) for q in range(3)])
            pTs = Rot(P, [sb("pTt%d" % q, [128, 4, 128], BF16) for q in range(13)])
            Obs = Rot(P, [sb("Ob%d" % q, [128, 8, 65], F32) for q in range(3)])
            self.bcast_load(gain_bc[:], self.mix_norm[i], bgain)
            P.op("gpsimd", lambda h: h.iota(reli[:], pattern=[[128, 3], [-1, 128]], base=-128, channel_multiplier=1), writes=[brel])
            P.op("vector", lambda h: h.tensor_copy(out=rel[:], in_=reli[:]), reads=[brel], writes=[brel])
            P.op("scalar", lambda h: h.activation(out=rel[:], in_=rel[:], func=AF.Abs), reads=[brel], writes=[brel])
            P.op("vector", lambda h: h.tensor_single_scalar(out=msk[:], in_=rel[:], scalar=64.5, op=ALU.is_le), reads=[brel], writes=[brel])
            P.op("gpsimd", lambda h: h.memset(V[:, :, :, 64:65], 1.0), writes=[bV])
            for g in range(3):
                d = dils[g]
                L = S // d
                nb = L // 128
                wsrc = self.at_w_qkv[j].rearrange("(k p) f -> p k f", p=128)
                self.cast_load(wq[:], wsrc[:, :, g * 512:(g + 1) * 512], bwq)
                self.cast_load(wk[:], wsrc[:, :, 1536 + g * 512:1536 + (g + 1) * 512], bwk)
                self.cast_load(wvv[:], wsrc[:, :, 3072 + g * 512:3072 + (g + 1) * 512], bwvv)
                for half in range(2):
                    P.dma("sync", lambda h, half=half, g=g: h.dma_start(out=gq[half * 64:(half + 1) * 64, :], in_=self.at_q_norm[j, g].rearrange("(p o) -> p o", o=1)), uwrites=[bg])
                    P.dma("sync", lambda h, half=half, g=g: h.dma_start(out=gk[half * 64:(half + 1) * 64, :], in_=self.at_k_norm[j, g].rearrange("(p o) -> p o", o=1)), uwrites=[bg])
                P.op("vector", lambda h: h.tensor_scalar(out=gq[:], in0=gq[:], scalar1=0.125, scalar2=None, op0=ALU.mult), reads=[bg], writes=[bg])
                for ei in range(8):
                    hd = 2 * (ei % 4) + ei // 4
                    slope = 2.0 ** (-(hd + 1))
                    P.op("scalar", lambda h, slope=slope, d=d: h.activation(out=etmp[:], in_=rel[:], func=AF.Exp, scale=-slope * d), reads=[brel], writes=[brel])
                    P.op("vector", lambda h, ei=ei: h.tensor_tensor(out=E[:, :, ei, :], in0=etmp[:], in1=msk[:], op=ALU.mult), reads=[brel], writes=[bE])
                stP = {}

                def stage_pn(pb, d=d, nb=nb):
                    hTb, bhTb = hTbs.next()
                    for q in range(4):
                        pt = pb * 4 + q
                        r, b = pt // nb, pt % nb
                        xt, bx = xts.next()
                        P.dma("sync", lambda h, xt=xt, r=r, b=b, d=d: h.dma_start(out=xt[:], in_=self.rows_ap(src, D, r + b * 128 * d, d)), writes=[bx])
                        hb, bhb = self.norm_tile(xt[:], bx, gain_bc, bgain, W)
                        self.transpose8(hb, bhb, hTb[:, :, q * 128:(q + 1) * 128], bhTb, W)
                    stP[pb] = (hTb, bhTb)

                def qk_finish(pend):
                    pq, bpq, sq, bsq, gt, dstT, bdst, c, pb = pend
                    pn, bpn = pns.next()
                    P.op("tensor", lambda h, pn=pn, sq=sq: h.matmul(pn[:], lhsT=self.blockones[:], rhs=sq[:], start=True, stop=True),
                         reads=[bsq, self.b_const], writes=[bpn])
                    sd, bsd = stds.next()
                    P.op("scalar", lambda h, sd=sd, pn=pn: h.activation(out=sd[:], in_=pn[:], func=AF.Ln, scale=1.0 / 64, bias=self.eps_t[:, 0:1]),
                         reads=[bpn, self.b_const], writes=[bsd])
                    P.op("scalar", lambda h, sd=sd: h.activation(out=sd[:], in_=sd[:], func=AF.Exp, scale=-0.5), reads=[bsd], writes=[bsd])
                    P.op("vector", lambda h, sd=sd, pq=pq, gt=gt, dstT=dstT, c=c, pb=pb: h.scalar_tensor_tensor(
                        out=dstT[:, c, pb * 512:(pb + 1) * 512], in0=pq[:], scalar=gt[:, 0:1], in1=sd[:], op0=ALU.mult, op1=ALU.mult),
                        reads=[bpq, bsd, bg], uwrites=[bdst])

                def stage_pm(pb):
                    hTb, bhTb = stP.pop(pb)
                    pend = None
                    for (wt, bwt, gt, dstT, bdst) in ((wq, bwq, gq, QT, bQT), (wk, bwk, gk, KT, bKT)):
                        for c in range(4):
                            pq, bpq = pqs.next()
                            for k in range(8):
                                P.op("tensor", lambda h, pq=pq, wt=wt, hTb=hTb, k=k, c=c: h.matmul(
                                    pq[:], lhsT=wt[:, k, c * 128:(c + 1) * 128], rhs=hTb[:, k, :], start=(k == 0), stop=(k == 7)),
                                    reads=[bwt, bhTb], writes=[bpq])
                            sq, bsq = sqs.next()
                            P.op("scalar", lambda h, sq=sq, pq=pq: h.activation(out=sq[:], in_=pq[:], func=AF.Square), reads=[bpq], writes=[bsq])
                            if pend is not None:
                                qk_finish(pend)
                            pend = (pq, bpq, sq, bsq, gt, dstT, bdst, c, pb)
                    for q in range(4):
                        pt = pb * 4 + q
                        pq, bpq = pqs.next()
                        for k in range(8):
                            P.op("tensor", lambda h, pq=pq, hTb=hTb, k=k, q=q: h.matmul(
                                pq[:], lhsT=hTb[:, k, q * 128:(q + 1) * 128], rhs=wvv[:, k, :], start=(k == 0), stop=(k == 7)),
                                reads=[bwvv, bhTb], writes=[bpq])
                        if pend is not None:
                            qk_finish(pend)
                            pend = None
                        P.op("vector", lambda h, pq=pq, pt=pt: h.tensor_copy(out=V[:, pt, :, 0:64], in_=pq[:].rearrange("p (a b) -> p a b", b=64)),
                             reads=[bpq], uwrites=[bV])

                stage_pn(0)
                for pb in range(8):
                    if pb + 1 < 8:
                        stage_pn(pb + 1)
                    stage_pm(pb)
                stA = {}

                def stage_s(qb, nb=nb):
                    r, b = qb // nb, qb % nb
                    offs = [o for o in (-1, 0, 1) if 0 <= b + o < nb]
                    pTl = {}
                    for hh in range(2):
                        for o in offs:
                            kt = qb + o
                            pS, bpS = pSs.next()
                            for m in range(4):
                                hd = 2 * m + hh
                                c, hp = hd // 2, hd % 2
                                P.op("tensor", lambda h, pS=pS, m=m, c=c, hp=hp, kt=kt, qb=qb: h.matmul(
                                    pS[:, m * 128:(m + 1) * 128], lhsT=KT[hp * 64:(hp + 1) * 64, c, kt * 128:(kt + 1) * 128],
                                    rhs=QT[hp * 64:(hp + 1) * 64, c, qb * 128:(qb + 1) * 128], start=True, stop=True),
                                    reads=[bKT, bQT], writes=[bpS])
                            ex, bex = exs.next()
                            P.op("scalar", lambda h, ex=ex, pS=pS: h.activation(out=ex[:].rearrange("p a b -> p (a b)"), in_=pS[:], func=AF.Exp), reads=[bpS], writes=[bex])
                            pT_, bpT_ = pTs.next()
                            P.op("vector", lambda h, ex=ex, pT_=pT_, o=o, hh=hh: h.tensor_tensor(
                                out=pT_[:], in0=ex[:], in1=E[:, o + 1, hh * 4:(hh + 1) * 4, :], op=ALU.mult), reads=[bex, bE], writes=[bpT_])
                            pTl[(hh, o)] = (pT_, bpT_)
                    stA[qb] = (offs, pTl)

                def stage_pv(qb, nb=nb, d=d, g=g):
                    r, b = qb // nb, qb % nb
                    offs, pTl = stA.pop(qb)
                    Ob, bOb = Obs.next()
                    for hh in range(2):
                        pO, bpO = pOs[hh], bpOs[hh]
                        for m in range(4):
                            hd = 2 * m + hh
                            for oi, o in enumerate(offs):
                                kt = qb + o
                                pT_, bpT_ = pTl[(hh, o)]
                                P.op("tensor", lambda h, pO=pO, pT_=pT_, m=m, kt=kt, hd=hd, oi=oi, n=len(offs): h.matmul(
                                    pO[:, m, :], lhsT=pT_[:, m, :], rhs=V[:, kt, hd, :], start=(oi == 0), stop=(oi == n - 1)),
                                    reads=[bpT_, bV], writes=[bpO])
                        P.op("vector" if hh == 0 else "scalar",
                             (lambda h, Ob=Ob, pO=pO, hh=hh: h.tensor_copy(out=Ob[:].rearrange("p (m b) c -> p m b c", b=2)[:, :, hh, :], in_=pO[:])) if hh == 0 else
                             (lambda h, Ob=Ob, pO=pO, hh=hh: h.copy(out=Ob[:].rearrange("p (m b) c -> p m b c", b=2)[:, :, hh, :], in_=pO[:])),
                             reads=[bpO], uwrites=[bOb])
                    P.dma("sync", lambda h, Ob=Ob, r=r, b=b, d=d, g=g: h.dma_start(
                        out=self.rows_ap(self.N_D[g], 520, r + b * 128 * d, d), in_=Ob[:].rearrange("p a b -> p (a b)")), reads=[bOb])

                stage_s(0)
                for qb in range(NT):
                    if qb + 1 < NT:
                        stage_s(qb + 1)
                    stage_pv(qb)
            P.end_phase()
        with self.phase() as st:
            sb = lambda n, s, d: st.enter_context(nc.sbuf_tensor("p%d_%s" % (self.pid, n), s, d))
            ps = lambda n, s, d: st.enter_context(nc.psum_tensor("p%d_%s" % (self.pid, n), s, d))
            wo = sb("wo", [128, 4, D], BF16); bwo = P.buf()
            self.cast_load(wo[:], self.at_w_o[j].rearrange("(c p) o -> p c o", p=128), bwo)
            Ns = [Rot(P, [sb("N%d_%d" % (g, q), [128, 8, 65], F32) for q in range(6)]) for g in range(3)]
            rds = Rot(P, [sb("rd%d" % q, [128, 8], F32) for q in range(4)])
            obs = Rot(P, [sb("ob%d" % q, [128, 8, 64], BF16) for q in range(4)])
            pT2 = Rot(P, [ps("pT2_%d" % q, [128, 4, 128], BF16) for q in range(2)])
            oTs = Rot(P, [sb("oT%d" % q, [128, 4, 128], BF16) for q in range(4)])
            xts = Rot(P, [sb("xt%d" % q, [128, D], F32) for q in range(7)])
            xns = Rot(P, [sb("xn%d" % q, [128, D], F32) for q in range(4)])
            pos = Rot(P, [ps("po%d" % q, [128, 512], F32) for q in range(4)])
            stG = {}

            stGL = {}

            def stage_gl(t):
                tl = []
                for g in range(3):
                    n_, bn = Ns[g].next()
                    P.dma("sync", lambda h, n_=n_, g=g, t=t: h.dma_start(out=n_[:].rearrange("p a b -> p (a b)"), in_=self.N_D[g][t * 128:(t + 1) * 128, :]), writes=[bn])
                    tl.append((n_, bn))
                xt, bx = xts.next()
                P.dma("sync", lambda h, xt=xt, t=t: h.dma_start(out=xt[:], in_=src[t * 128:(t + 1) * 128, :]), writes=[bx])
                stGL[t] = (tl, xt, bx)

            def stage_g0(t):
                tl, xt, bx = stGL.pop(t)
                n0, bn0 = tl[0]
                P.op("vector", lambda h, n0=n0, n1=tl[1][0]: h.tensor_tensor(out=n0[:], in0=n0[:], in1=n1[:], op=ALU.add), reads=[tl[1][1], bn0], writes=[bn0])
                P.op("vector", lambda h, n0=n0, n2=tl[2][0]: h.tensor_tensor(out=n0[:], in0=n0[:], in1=n2[:], op=ALU.add), reads=[tl[2][1], bn0], writes=[bn0])
                rd, brd = rds.next()
                P.op("vector", lambda h, rd=rd, n0=n0: h.reciprocal(out=rd[:], in_=n0[:, :, 64]), reads=[bn0], writes=[brd])
                ob, bob = obs.next()
                P.op("vector", lambda h, ob=ob, n0=n0, rd=rd: h.tensor_tensor(
                    out=ob[:], in0=n0[:, :, 0:64], in1=rd[:].unsqueeze(2).to_broadcast([128, 8, 64]), op=ALU.mult),
                    reads=[bn0, brd], writes=[bob])
                stG[t] = dict(xt=xt, bx=bx, ob=ob, bob=bob)

            def stage_g1(t):
                c_ = stG[t]
                ob, bob = c_["ob"], c_["bob"]
                pt_, bpt = pT2.next()
                for c in range(4):
                    P.op("tensor", lambda h, pt_=pt_, ob=ob, c=c: h.transpose(
                        out=pt_[:, c, :], in_=ob[:, 2 * c:2 * c + 2, :].rearrange("p a b -> p (a b)"), identity=self.ident_bf[:]),
                        reads=[bob, self.b_const], writes=[bpt])
                oT, boT = oTs.next()
                P.op("scalar", lambda h, oT=oT, pt_=pt_: h.copy(out=oT[:], in_=pt_[:]), reads=[bpt], writes=[boT])
                c_["oT"], c_["boT"] = oT, boT

            def stage_g2(t):
                c_ = stG.pop(t)
                xt, bx, oT, boT = c_["xt"], c_["bx"], c_["oT"], c_["boT"]
                xn, bxn = xns.next()
                for obk in range(2):
                    po, bpo = pos.next()
                    for c in range(4):
                        P.op("tensor", lambda h, po=po, oT=oT, c=c, obk=obk: h.matmul(
                            po[:], lhsT=oT[:, c, :], rhs=wo[:, c, obk * 512:(obk + 1) * 512], start=(c == 0), stop=(c == 3)),
                            reads=[boT, bwo], writes=[bpo])
                    P.op("vector", lambda h, po=po, xn=xn, xt=xt, obk=obk: h.tensor_tensor(
                        out=xn[:, obk * 512:(obk + 1) * 512], in0=po[:], in1=xt[:, obk * 512:(obk + 1) * 512], op=ALU.add),
                        reads=[bpo, bx], uwrites=[bxn])
                P.dma("sync", lambda h, xn=xn, t=t: h.dma_start(out=self.out[t * 128:(t + 1) * 128, :], in_=xn[:]), reads=[bxn])

            for step in range(NT + 4):
                if step < NT:
                    stage_gl(step)
                if 0 <= step - 2 < NT:
                    stage_g0(step - 2)
                if 0 <= step - 3 < NT:
                    stage_g1(step - 3)
                if 0 <= step - 4 < NT:
                    stage_g2(step - 4)
            P.end_phase()

    def moe(self, i):
        nc, P = self.nc, self.P
        src = self.out
        with self.phase() as st:
            sb = lambda n, s, d: st.enter_context(nc.sbuf_tensor("p%d_%s" % (self.pid, n), s, d))
            ps = lambda n, s, d: st.enter_context(nc.psum_tensor("p%d_%s" % (self.pid, n), s, d))
            gain_bc = sb("gain_bc", [128, D], F32); bgain = P.buf()
            self.eps_t = sb("eps_t", [128, 1], F32)
            P.op("vector", lambda h: h.memset(self.eps_t[:], EPS), reads=[self.b_const], writes=[self.b_const])
            wr = sb("wr", [128, 8, 16], F32); bwr = P.buf()
            br_bc = sb("br_bc", [128, 16], F32); bbr = P.buf()
            affT = sb("affT", [16, S], F32); baffT = P.buf()
            cjunk = sb("cjunk", [128, S], BF16); bcj = P.buf()
            ajunk = sb("ajunk", [128, S], BF16); baj = P.buf()
            lo = sb("lo", [16, 4], F32); blo = P.buf()
            jvi = sb("jvi", [128, 4], I32); jv = sb("jv", [128, 4], F32); jvh = sb("jvh", [128, 4], F32); bjv = P.buf()
            idxf = sb("idxf", [128, 64], F32); idxi = sb("idxi", [128, 64], I32); bidx = P.buf()
            xts = Rot(P, [sb("xt%d" % q, [128, D], F32) for q in range(5)])
            junks = Rot(P, [sb("junk%d" % q, [128, D], BF16) for q in range(2)])
            stats = Rot(P, [sb("stat%d" % q, [128, 8], F32) for q in range(6)])
            hfs = Rot(P, [sb("hf%d" % q, [128, D], F32) for q in range(4)])
            hbs = Rot(P, [sb("hbx%d" % q, [128, 1056], BF16) for q in range(6)])
            pTfs = Rot(P, [ps("pTf%d" % q, [128, 4, 128], F32) for q in range(4)])
            hT32s = Rot(P, [sb("hT32_%d" % q, [128, 8, 128], F32) for q in range(3)])
            prs = Rot(P, [ps("pr%d" % q, [128, 16], F32) for q in range(2)])
            lgs = Rot(P, [sb("lg%d" % q, [128, 16], F32) for q in range(6)])
            pats = Rot(P, [ps("pat%d" % q, [16, 128], F32) for q in range(2)])
            cbcs = Rot(P, [sb("cbc%d" % q, [128, S], F32) for q in range(2)])
            self.bcast_load(gain_bc[:], self.ffn_norm[i], bgain)
            self.bcast_load(br_bc[:], self.moe_b_router[i], bbr)
            P.dma("sync", lambda h: h.dma_start(out=wr[:], in_=self.moe_w_router[i].rearrange("(k p) e -> p k e", p=128)), writes=[bwr])
            stR = {}

            stRL = {}

            def stage_rl(t):
                xt, bx = xts.next()
                P.dma("sync", lambda h, xt=xt, t=t: h.dma_start(out=xt[:], in_=src[t * 128:(t + 1) * 128, :]), writes=[bx])
                stRL[t] = (xt, bx)

            def stage_r0(t):
                xt, bx = stRL.pop(t)
                junk, bjunk = junks.next()
                stt, bst = stats.next()
                hf, bhf = hfs.next()
                P.op("scalar", lambda h, junk=junk, xt=xt, stt=stt: h.activation(out=junk[:], in_=xt[:], func=AF.Square, accum_out=stt[:, 0:1]),
                     reads=[bx], writes=[bjunk, bst])
                P.op("scalar", lambda h, stt=stt: h.activation(out=stt[:, 1:2], in_=stt[:, 0:1], func=AF.Ln, scale=1.0 / D, bias=self.eps_t[:, 0:1]),
                     reads=[bst, self.b_const], writes=[bst])
                P.op("scalar", lambda h, stt=stt: h.activation(out=stt[:, 2:3], in_=stt[:, 1:2], func=AF.Exp, scale=-0.5), reads=[bst], writes=[bst])
                P.op("vector", lambda h, hf=hf, xt=xt, stt=stt: h.scalar_tensor_tensor(out=hf[:], in0=xt[:], scalar=stt[:, 2:3], in1=gain_bc[:], op0=ALU.mult, op1=ALU.mult),
                     reads=[bx, bst, bgain], writes=[bhf])
                hb, bhb = hbs.next()
                P.op("gpsimd", lambda h, hb=hb, hf=hf: h.tensor_copy(out=hb[:, 0:D], in_=hf[:]), reads=[bhf], uwrites=[bhb])
                stR[t] = dict(stt=stt, bst=bst, hf=hf, bhf=bhf, hb=hb, bhb=bhb)

            def stage_r1(t):
                c = stR[t]
                hf, bhf = c["hf"], c["bhf"]
                hT32, bhT32 = hT32s.next()
                for half in range(2):
                    pTf, bpTf = pTfs.next()
                    for k in range(4):
                        kk = half * 4 + k
                        P.op("tensor", lambda h, pTf=pTf, hf=hf, k=k, kk=kk: h.transpose(out=pTf[:, k, :], in_=hf[:, kk * 128:(kk + 1) * 128], identity=self.ident_f[:]),
                             reads=[bhf, self.b_const], writes=[bpTf])
                    if half == 0:
                        P.op("vector", lambda h, hT32=hT32, pTf=pTf: h.tensor_copy(out=hT32[:, 0:4, :], in_=pTf[:]), reads=[bpTf], uwrites=[bhT32])
                    else:
                        P.op("scalar", lambda h, hT32=hT32, pTf=pTf: h.copy(out=hT32[:, 4:8, :], in_=pTf[:]), reads=[bpTf], uwrites=[bhT32])
                c["hT32"], c["bhT32"] = hT32, bhT32

            def stage_r2(t):
                c = stR[t]
                stt, bst, hb, bhb, hT32, bhT32 = c["stt"], c["bst"], c["hb"], c["bhb"], c["hT32"], c["bhT32"]
                pr, bpr = prs.next()
                for k in range(8):
                    P.op("tensor", lambda h, pr=pr, hT32=hT32, k=k: h.matmul(pr[:], lhsT=hT32[:, k, :], rhs=wr[:, k, :], start=(k == 0), stop=(k == 7)),
                         reads=[bhT32, bwr], writes=[bpr])
                lg, blg = lgs.next()
                P.op("vector", lambda h, lg=lg, pr=pr: h.tensor_tensor(out=lg[:], in0=pr[:], in1=br_bc[:], op=ALU.add), reads=[bpr, bbr], writes=[blg])
                P.op("vector", lambda h, lg=lg, stt=stt: h.reduce_max(out=stt[:, 3:4], in_=lg[:], axis=mybir.AxisListType.X), reads=[blg, bst], writes=[bst])
                P.op("vector", lambda h, stt=stt: h.tensor_scalar(out=stt[:, 4:5], in0=stt[:, 3:4], scalar1=-1.0, scalar2=None, op0=ALU.mult), reads=[bst], writes=[bst])
                P.op("scalar", lambda h, lg=lg, stt=stt: h.activation(out=lg[:], in_=lg[:], func=AF.Exp, bias=stt[:, 4:5], accum_out=stt[:, 5:6]),
                     reads=[blg, bst], writes=[blg, bst])
                P.op("vector", lambda h, stt=stt: h.reciprocal(out=stt[:, 6:7], in_=stt[:, 5:6]), reads=[bst], writes=[bst])
                P.op("vector", lambda h, lg=lg, stt=stt: h.tensor_scalar(out=lg[:], in0=lg[:], scalar1=stt[:, 6:7], scalar2=None, op0=ALU.mult), reads=[blg, bst], writes=[blg])
                P.op("gpsimd", lambda h, hb=hb, lg=lg: h.tensor_copy(out=hb[:, D:1056].bitcast(F32), in_=lg[:]), reads=[blg], uwrites=[bhb])
                P.dma("sync", lambda h, hb=hb, t=t: h.dma_start(out=self.hD[t * 128:(t + 1) * 128, :], in_=hb[:]), reads=[bhb])
                c["lg"], c["blg"] = lg, blg

            def stage_r3(t):
                c = stR.pop(t)
                lg, blg = c["lg"], c["blg"]
                pat, bpat = pats.next()
                P.op("tensor", lambda h, pat=pat, lg=lg: h.transpose(out=pat[:], in_=lg[:], identity=self.ident_f[:]), reads=[blg, self.b_const], writes=[bpat])
                P.op("scalar", lambda h, pat=pat, t=t: h.copy(out=affT[:, t * 128:(t + 1) * 128], in_=pat[:]), reads=[bpat], uwrites=[baffT])

            for step in range(NT + 5):
                if step < NT:
                    stage_rl(step)
                if 0 <= step - 2 < NT:
                    stage_r0(step - 2)
                if 0 <= step - 3 < NT:
                    stage_r1(step - 3)
                if 0 <= step - 4 < NT:
                    stage_r2(step - 4)
                if 0 <= step - 5 < NT:
                    stage_r3(step - 5)
            P.op("vector", lambda h: h.memset(lo[:], 0.0), writes=[blo])
            for it in range(30):
                hstep = 2.0 ** (-(it + 1))
                P.op("vector", lambda h, hstep=hstep: h.tensor_scalar(out=lo[:, 1:2], in0=lo[:, 0:1], scalar1=hstep, scalar2=None, op0=ALU.add), reads=[blo], writes=[blo])
                P.op("vector", lambda h: h.tensor_scalar(out=cjunk[0:16, :], in0=affT[:], scalar1=lo[:, 1:2], scalar2=0.0, op0=ALU.is_ge, op1=ALU.add, accum_out=lo[:, 2:3]),
                     reads=[baffT, blo], writes=[bcj, blo])
                P.op("vector", lambda h, hstep=hstep: h.tensor_scalar(out=lo[:, 3:4], in0=lo[:, 2:3], scalar1=511.5, scalar2=hstep, op0=ALU.is_ge, op1=ALU.mult), reads=[blo], writes=[blo])
                P.op("vector", lambda h: h.tensor_tensor(out=lo[:, 0:1], in0=lo[:, 0:1], in1=lo[:, 3:4], op=ALU.add), reads=[blo], writes=[blo])
            P.op("vector", lambda h: h.tensor_scalar(out=affT[:], in0=affT[:], scalar1=lo[:, 0:1], scalar2=None, op0=ALU.is_ge), reads=[baffT, blo], writes=[baffT])
            P.op("vector", lambda h: h.tensor_tensor_scan(out=affT[:], data0=affT[:], data1=affT[:], initial=0.0, op0=ALU.add, op1=ALU.bypass), reads=[baffT], writes=[baffT])
            bcD = P.buf()
            P.dma("sync", lambda h: h.dma_start(out=self.cD.ap(), in_=affT[:]), reads=[baffT], writes=[bcD])
            P.op("gpsimd", lambda h: h.iota(jvi[:], pattern=[[128, 4]], base=0, channel_multiplier=1), writes=[bjv])
            P.op("vector", lambda h: h.tensor_copy(out=jv[:], in_=jvi[:]), reads=[bjv], writes=[bjv])
            P.op("vector", lambda h: h.tensor_scalar(out=jvh[:], in0=jv[:], scalar1=0.5, scalar2=None, op0=ALU.add), reads=[bjv], writes=[bjv])
            for e in range(16):
                cbc, bcbc = cbcs.next()
                P.dma("sync", lambda h, cbc=cbc, e=e: h.dma_start(out=cbc[:], in_=self.cD[e].partition_broadcast(128)), reads=[bcD], writes=[bcbc])
                for J in range(4):
                    col = e * 4 + J
                    if J < 2:
                        P.op("vector", lambda h, cbc=cbc, J=J, col=col: h.tensor_scalar(
                            out=cjunk[:], in0=cbc[:], scalar1=jv[:, J:J + 1], scalar2=0.0, op0=ALU.is_le, op1=ALU.add, accum_out=idxf[:, col:col + 1]),
                            reads=[bcbc, bjv], writes=[bcj], uwrites=[bidx])
                    else:
                        P.op("scalar", lambda h, cbc=cbc, J=J, col=col: h.activation(
                            out=ajunk[:], in_=cbc[:], func=AF.Sign, scale=-1.0, bias=jvh[:, J:J + 1], accum_out=idxf[:, col:col + 1]),
                            reads=[bcbc, bjv], writes=[baj], uwrites=[bidx])
            iv = idxf[:].rearrange("p (e j) -> p e j", j=4)
            P.op("vector", lambda h: h.tensor_scalar(out=iv[:, :, 2:4], in0=iv[:, :, 2:4], scalar1=0.5, scalar2=2048.0, op0=ALU.mult, op1=ALU.add), reads=[bidx], writes=[bidx])
            P.op("vector", lambda h: h.tensor_copy(out=idxi[:], in_=idxf[:]), reads=[bidx], writes=[bidx])
            P.dma("sync", lambda h: h.dma_start(out=self.idxD.ap(), in_=idxi[:]), reads=[bidx])
            P.end_phase()
        with self.phase() as st:
            sb = lambda n, s, d: st.enter_context(nc.sbuf_tensor("p%d_%s" % (self.pid, n), s, d))
            ps = lambda n, s, d: st.enter_context(nc.psum_tensor("p%d_%s" % (self.pid, n), s, d))
            NS = 8
            ring = [sb("ring%d" % q, [128, 8, 1024], BF16) for q in range(NS)]
            bring = [P.buf() for _ in range(NS)]
            idx = sb("idx", [128, 64], I32); bidx = P.buf()
            xins = Rot(P, [sb("xin%d" % q, [128, 4, 1056], BF16) for q in range(2)])
            xinTs = Rot(P, [sb("xinT%d" % q, [128, 8, 512], BF16) for q in range(1)])
            hidTs = Rot(P, [sb("hidT", [128, 16, 512], BF16)])
            sls = Rot(P, [sb("sl%d" % q, [128, 512], BF16) for q in range(2)])
            ybs = Rot(P, [sb("yb%d" % q, [128, D], F32) for q in range(2)])
            pTs_ = Rot(P, [ps("pTx%d" % q, [128, 8, 128], BF16) for q in range(2)])
            pAs = Rot(P, [ps("pA%d" % q, [128, 512], F32) for q in range(2)])
            pBs = Rot(P, [ps("pB%d" % q, [128, 512], F32) for q in range(2)])
            pYs = Rot(P, [ps("pY%d" % q, [128, 512], F32) for q in range(2)])
            bout = P.buf()
            regbox = {}
            P.dma("sync", lambda h: h.dma_start(out=idx[:], in_=self.idxD.ap()), writes=[bidx])

            def unit_src(e, u):
                if u < 4:
                    w = self.moe_w1 if u % 2 == 0 else self.moe_w3
                    hf = u // 2
                    return w[i, e, :, hf * 1024:(hf + 1) * 1024].rearrange("(k p) f -> p k f", p=128)
                hf = u - 4
                return self.moe_w2[i, e, hf * 1024:(hf + 1) * 1024, :].rearrange("(c p) o -> p c o", p=128)

            def load_unit(e, u):
                s = (6 * e + u) % NS
                self.cast_load(ring[s][:], unit_src(e, u), bring[s])

            def gather(e):
                xin, bxin = xins.next()
                for J in range(4):
                    col = e * 4 + J
                    P.dma("gpsimd", lambda h, xin=xin, J=J, col=col: h.indirect_dma_start(
                        out=xin[:, J, :], out_offset=None, in_=self.hD.ap(),
                        in_offset=bass.IndirectOffsetOnAxis(ap=idx[:, col:col + 1], axis=0)), reads=[bidx], uwrites=[bxin])
                return xin, bxin

            nxt = gather(0)
            for u in range(6):
                load_unit(0, u)
            for e in range(16):
                xin, bxin = nxt
                if e + 1 < 16:
                    nxt = gather(e + 1)
                xinT, bxT = xinTs.next()
                for J in range(4):
                    pT_, bpT_ = pTs_.next()
                    for k in range(8):
                        P.op("tensor", lambda h, pT_=pT_, xin=xin, J=J, k=k: h.transpose(out=pT_[:, k, :], in_=xin[:, J, k * 128:(k + 1) * 128], identity=self.ident_bf[:]),
                             reads=[bxin, self.b_const], writes=[bpT_])
                    if J % 2 == 0:
                        P.op("vector", lambda h, pT_=pT_, xinT=xinT, J=J: h.tensor_copy(out=xinT[:, :, J * 128:(J + 1) * 128], in_=pT_[:]), reads=[bpT_], uwrites=[bxT])
                    else:
                        P.op("scalar", lambda h, pT_=pT_, xinT=xinT, J=J: h.copy(out=xinT[:, :, J * 128:(J + 1) * 128], in_=pT_[:]), reads=[bpT_], uwrites=[bxT])
                hidT, bhid = hidTs.next()
                for hf in range(2):
                    s1 = (6 * e + 2 * hf) % NS
                    s3 = (6 * e + 2 * hf + 1) % NS
                    for fcl in range(8):
                        fc = hf * 8 + fcl
                        pA, bpA = pAs.next()
                        pB, bpB = pBs.next()
                        for k in range(8):
                            P.op("tensor", lambda h, pA=pA, s1=s1, k=k, fcl=fcl, xinT=xinT: h.matmul(
                                pA[:], lhsT=ring[s1][:, k, fcl * 128:(fcl + 1) * 128], rhs=xinT[:, k, :], start=(k == 0), stop=(k == 7)),
                                reads=[bring[s1], bxT], writes=[bpA])
                        for k in range(8):
                            P.op("tensor", lambda h, pB=pB, s3=s3, k=k, fcl=fcl, xinT=xinT: h.matmul(
                                pB[:], lhsT=ring[s3][:, k, fcl * 128:(fcl + 1) * 128], rhs=xinT[:, k, :], start=(k == 0), stop=(k == 7)),
                                reads=[bring[s3], bxT], writes=[bpB])
                        sl, bsl = sls.next()
                        P.op("scalar", lambda h, sl=sl, pA=pA: h.activation(out=sl[:], in_=pA[:], func=AF.Silu), reads=[bpA], writes=[bsl])
                        P.op("vector", lambda h, sl=sl, pB=pB, hidT=hidT, fc=fc: h.tensor_tensor(out=hidT[:, fc, :], in0=pB[:], in1=sl[:], op=ALU.mult),
                             reads=[bpB, bsl], uwrites=[bhid])
                    if e + 1 < 16:
                        if hf == 0:
                            load_unit(e + 1, 0)
                            load_unit(e + 1, 1)
                            load_unit(e + 1, 2)
                            load_unit(e + 1, 3)
                        else:
                            load_unit(e + 1, 4)
                            load_unit(e + 1, 5)
                sA = (6 * e + 4) % NS
                sB = (6 * e + 5) % NS
                for J in range(4):
                    yb, byb = ybs.next()
                    for ob in range(2):
                        pY, bpY = pYs.next()
                        for fc in range(16):
                            sw = sA if fc < 8 else sB
                            P.op("tensor", lambda h, pY=pY, hidT=hidT, fc=fc, J=J, ob=ob, sw=sw: h.matmul(
                                pY[:], lhsT=hidT[:, fc, J * 128:(J + 1) * 128], rhs=ring[sw][:, fc % 8, ob * 512:(ob + 1) * 512], start=(fc == 0), stop=(fc == 15)),
                                reads=[bhid, bring[sw]], writes=[bpY])
                        gate = xin[:, J, D:1056].bitcast(F32)[:, e:e + 1]
                        if ob == 0:
                            P.op("vector", lambda h, yb=yb, pY=pY, gate=gate, ob=ob: h.tensor_scalar(out=yb[:, ob * 512:(ob + 1) * 512], in0=pY[:], scalar1=gate, scalar2=None, op0=ALU.mult),
                                 reads=[bpY, bxin], uwrites=[byb])
                        else:
                            P.op("scalar", lambda h, yb=yb, pY=pY, gate=gate, ob=ob: h.activation(out=yb[:, ob * 512:(ob + 1) * 512], in_=pY[:], func=AF.Copy, scale=gate),
                                 reads=[bpY, bxin], uwrites=[byb])
                    col = e * 4 + J
                    def scat(h, yb=yb, col=col):
                        if "r" not in regbox:
                            regbox["r"] = h.to_reg(S - 1)
                        return h.indirect_dma_start(
                            out=self.out.ap(), out_offset=bass.IndirectOffsetOnAxis(ap=idx[:, col:col + 1], axis=0), in_=yb[:], in_offset=None,
                            bounds_check=regbox["r"], oob_is_err=True, compute_op=ALU.add)
                    P.dma("gpsimd", scat, reads=[byb, bidx], writes=[bout])
            P.end_phase()

    def build(self):
        P = self.P
        self.consts()
        first = True
        for i in self.layers:
            if "mix" in self.sub:
                src = self.x_in if first else self.out
                if i % 2 == 0:
                    self.gmlp(i, src)
                else:
                    self.attn(i, src)
                first = False
            if "moe" in self.sub:
                if first:
                    with self.phase() as st:
                        t = st.enter_context(self.nc.sbuf_tensor("cp%d" % self.pid, [128, 8, D], F32))
                        b = P.buf()
                        for q in range(4):
                            P.dma("sync", lambda h, q=q: h.dma_start(out=t[:], in_=self.x_in[q * 1024:(q + 1) * 1024, :].rearrange("(a p) d -> p a d", p=128)), writes=[b])
                            P.dma("sync", lambda h, q=q: h.dma_start(out=self.out[q * 1024:(q + 1) * 1024, :].rearrange("(a p) d -> p a d", p=128), in_=t[:]), reads=[b])
                        P.end_phase()
                    first = False
                self.moe(i)


_WNAMES = ["mix_norm", "ffn_norm", "gm_w_in", "gm_b_in", "gm_v_norm", "gm_w_s", "gm_b_s", "gm_w_out", "gm_b_out",
           "at_w_qkv", "at_q_norm", "at_k_norm", "at_w_o", "moe_w_router", "moe_b_router", "moe_w1", "moe_w3", "moe_w2"]


def build_nc(layers=(0, 1, 2, 3), sub=("mix", "moe")):
    nc = bass.Bass("TRN2", target_bir_lowering=False)
    with contextlib.ExitStack() as stack:
        k = K(nc, stack, layers, sub)
        k.build()
    return nc


def kernel(**inputs):
    x = np.ascontiguousarray(inputs["x"], dtype=np.float32)
    B = x.shape[0]
    w = {n: np.ascontiguousarray(inputs[n], dtype=np.float32) for n in _WNAMES}
    nc = build_nc()
    in_maps = []
    for b in range(B):
        m = {"x": x[b]}
        m.update(w)
        in_maps.append(m)
    res = run_bass_kernel_spmd(nc, in_maps, core_ids=list(range(B)))
    return np.stack([np.asarray(r["out"]) for r in res.results], axis=0).astype(np.float32)
```
